# Optimizing a Trainium2 kernel written in Bass

```python
import math
import jax
import jax.numpy as jnp
from jax import lax
import numpy as np

D_MODEL = 2048
BATCH = 2
SEQ = 4096
DEPTH = 2

GRID_W = 64
CTX_LEN = 256
NORM_EPS = 1e-6
N_BRANCH = 3

S5_WIDTH = 1024
S5_GROUP = 16
S5_GROUPS = S5_WIDTH // S5_GROUP
S5_STATE = 64
S5_DT_MIN = 1e-3
S5_DT_MAX = 1e-1
S5_MAX_RE = -1e-4

RW_WIDTH = 1024
RW_HEAD = 64
RW_HEADS = RW_WIDTH // RW_HEAD
RW_DECAY_RANK = 64
RW_ICLR_RANK = 64
RW_GATE_RANK = 128
RW_DECAY_SCALE = 0.606531
RW_GN_EPS = 64e-5
RW_SHIFT_WIDTH = 3 * RW_WIDTH + RW_DECAY_RANK + RW_ICLR_RANK

GLA_HEADS = 4
GLA_DK = 128
GLA_DV = 256
GLA_QK_WIDTH = GLA_HEADS * GLA_DK
GLA_V_WIDTH = GLA_HEADS * GLA_DV
GLA_GATE_RANK = 16
GLA_TAU = 16.0
GLA_CHUNK = 64

MOE_GROUPS = 4
MOE_PER_GROUP = 8
MOE_EXPERTS = MOE_GROUPS * MOE_PER_GROUP
MOE_TOPK = 2
MOE_HIDDEN = 256

IN_SPLITS = (S5_WIDTH, RW_SHIFT_WIDTH, RW_GATE_RANK, GLA_QK_WIDTH, GLA_QK_WIDTH, GLA_V_WIDTH,
             GLA_GATE_RANK, GLA_V_WIDTH, N_BRANCH * D_MODEL)
IN_WIDTH = sum(IN_SPLITS)

kernel_name = 'hybrid_s5_rwkv7_gla_hmoe_prefix'


def _split_points():
    points, acc = [], 0
    for w in IN_SPLITS[:-1]:
        acc += w
        points.append(acc)
    return points


def _rmsnorm(x, g):
    xf = x.astype(jnp.float32)
    y = xf * lax.rsqrt(jnp.mean(xf * xf, axis=-1, keepdims=True) + NORM_EPS)
    return (y * g.astype(jnp.float32)).astype(x.dtype)


def _modulate(h, shift, scale):
    return h * (1.0 + scale) + shift


def _to_column_major(t, rows):
    bsz, n, d = t.shape
    return jnp.swapaxes(t.reshape(bsz, rows, GRID_W, d), 1, 2).reshape(bsz, n, d)


def _to_row_major(t, rows):
    bsz, n, d = t.shape
    return jnp.swapaxes(t.reshape(bsz, GRID_W, rows, d), 1, 2).reshape(bsz, n, d)


def _bidirectional(direction_fn, prm_fwd, prm_bwd, ctx_seq, lat_seq):
    def flip(seq):
        return tuple(jnp.flip(a, axis=1) for a in seq)
    y_cf, s_cf = direction_fn(ctx_seq, prm_fwd, None)
    y_lf, _ = direction_fn(lat_seq, prm_fwd, s_cf)
    y_cb, s_cb = direction_fn(flip(ctx_seq), prm_bwd, None)
    y_lb, _ = direction_fn(flip(lat_seq), prm_bwd, s_cb)
    return y_cf + jnp.flip(y_cb, axis=1), y_lf + jnp.flip(y_lb, axis=1)


def _linear_combine(left, right):
    a_l, b_l = left
    a_r, b_r = right
    return a_l * a_r, a_r * b_l + b_r


def _s5_direction(seq, prm, h0):
    (u,) = seq
    a_re, a_im, log_dt, b_re, b_im, c_re, c_im = (p.astype(jnp.float32) for p in prm)
    bsz, n, _ = u.shape
    lam = lax.complex(jnp.minimum(a_re, S5_MAX_RE), a_im)
    lam_bar = jnp.exp(lam * jnp.exp(log_dt)[:, None])
    b_bar = ((lam_bar - 1.0) / lam)[..., None] * lax.complex(b_re, b_im)
    c_mat = lax.complex(c_re, c_im)
    ug = u.astype(jnp.float32).reshape(bsz, n, S5_GROUPS, S5_GROUP).astype(jnp.complex64)
    bu = jnp.einsum('gpi,btgi->btgp', b_bar, ug)
    if h0 is not None:
        bu = bu.at[:, 0].add(lam_bar * h0)
    _, h = lax.associative_scan(_linear_combine, (jnp.broadcast_to(lam_bar, bu.shape), bu), axis=1)
    y = jnp.einsum('gip,btgp->btgi', c_mat, h).real.reshape(bsz, n, S5_WIDTH)
    return y, h[:, -1]


def _s5_out(y, u, d, w_glu):
    z = jax.nn.gelu(y + d.astype(jnp.float32) * u.astype(jnp.float32)).astype(u.dtype)
    val, gate = jnp.split(z @ w_glu, 2, axis=-1)
    return val * jax.nn.sigmoid(gate)


def _rwkv_direction(seq, prm, s0):
    (z,) = seq
    mu, w0, w_up, a0, a_up, k_k, k_a, r_k = (p.astype(jnp.float32) for p in prm)
    z = z.astype(jnp.float32)
    bsz, n, _ = z.shape
    prev = jnp.pad(z, ((0, 0), (1, 0), (0, 0)))[:, :-1]
    z = z + (prev - z) * mu
    r, k, v, w_lo, a_lo = jnp.split(
        z, [RW_WIDTH, 2 * RW_WIDTH, 3 * RW_WIDTH, 3 * RW_WIDTH + RW_DECAY_RANK], axis=-1)
    decay = jnp.exp(-RW_DECAY_SCALE * jax.nn.sigmoid(w0 + jnp.tanh(w_lo) @ w_up))
    a = jax.nn.sigmoid(a0 + a_lo @ a_up)
    kk = (k * k_k).reshape(bsz, n, RW_HEADS, RW_HEAD)
    kk = kk / jnp.maximum(jnp.sqrt(jnp.sum(kk * kk, axis=-1, keepdims=True)), 1e-12)
    k = k * (1.0 + (a - 1.0) * k_a)
    r, k, v, decay, a = (t.reshape(bsz, n, RW_HEADS, RW_HEAD) for t in (r, k, v, decay, a))
    if s0 is None:
        s0 = jnp.zeros((bsz, RW_HEADS, RW_HEAD, RW_HEAD), jnp.float32)

    def step(state, inp):
        r_t, k_t, v_t, w_t, kk_t, a_t = inp
        sa = jnp.einsum('bhvk,bhk->bhv', state, kk_t)
        state = (state * w_t[:, :, None, :] - sa[..., None] * (kk_t * a_t)[:, :, None, :]
                 + v_t[..., None] * k_t[:, :, None, :])
        return state, jnp.einsum('bhvk,bhk->bhv', state, r_t)

    s_fin, y = lax.scan(step, s0, tuple(jnp.swapaxes(t, 0, 1) for t in (r, k, v, decay, kk, a)))
    y = jnp.swapaxes(y, 0, 1) + jnp.sum(r * k * r_k, axis=-1, keepdims=True) * v
    return y.reshape(bsz, n, RW_WIDTH), s_fin


def _rwkv_out(y, g_lo, g_up, ln_w, ln_b, w_proj):
    bsz, n, _ = y.shape
    yh = y.reshape(bsz, n, RW_HEADS, RW_HEAD)
    mean = jnp.mean(yh, axis=-1, keepdims=True)
    var = jnp.mean(jnp.square(yh - mean), axis=-1, keepdims=True)
    yn = ((yh - mean) * lax.rsqrt(var + RW_GN_EPS)).reshape(bsz, n, RW_WIDTH)
    yn = yn * ln_w.astype(jnp.float32) + ln_b.astype(jnp.float32)
    gate = jax.nn.sigmoid(g_lo.astype(jnp.float32)) @ g_up.astype(jnp.float32)
    return (yn * gate).astype(g_lo.dtype) @ w_proj


def _gla_direction(seq, prm, s0):
    q, k, v, gk_lo = seq
    gk_up, gk_b = (p.astype(jnp.float32) for p in prm)
    bsz, n, _ = q.shape
    n_chunk = n // GLA_CHUNK
    log_a = jax.nn.log_sigmoid(gk_lo.astype(jnp.float32) @ gk_up + gk_b) / GLA_TAU

    def chunks(t, d):
        return t.astype(jnp.float32).reshape(bsz, n_chunk, GLA_CHUNK, GLA_HEADS, d)

    q = chunks(q, GLA_DK) * GLA_DK ** -0.5
    k = chunks(k, GLA_DK)
    v = chunks(v, GLA_DV)
    b = jnp.cumsum(chunks(log_a, GLA_DK), axis=2)
    b_ref = b[:, :, GLA_CHUNK // 2:GLA_CHUNK // 2 + 1]
    b_last = b[:, :, -1]
    scores = jnp.einsum('bnchd,bnmhd->bnhcm', q * jnp.exp(b - b_ref), k * jnp.exp(b_ref - b))
    lower = jnp.tril(jnp.ones((GLA_CHUNK, GLA_CHUNK), dtype=bool))
    scores = jnp.where(lower, scores, 0.0)
    o = jnp.einsum('bnhcm,bnmhe->bnche', scores, v)
    u_chunk = jnp.einsum('bnchd,bnche->bnhde', k * jnp.exp(b_last[:, :, None] - b), v)
    if s0 is None:
        s0 = jnp.zeros((bsz, GLA_HEADS, GLA_DK, GLA_DV), jnp.float32)

    def step(state, inp):
        dec, uc = inp
        return dec[..., None] * state + uc, state

    s_fin, s_prev = lax.scan(step, s0, (jnp.swapaxes(jnp.exp(b_last), 0, 1), jnp.swapaxes(u_chunk, 0, 1)))
    o = o + jnp.einsum('bnchd,bnhde->bnche', q * jnp.exp(b), jnp.swapaxes(s_prev, 0, 1))
    return o.reshape(bsz, n, GLA_V_WIDTH), s_fin


def _gla_out(o, og, norm_g, w_proj):
    bsz, n, _ = o.shape
    oh = o.reshape(bsz, n, GLA_HEADS, GLA_DV)
    oh = oh * lax.rsqrt(jnp.mean(oh * oh, axis=-1, keepdims=True) + NORM_EPS)
    on = oh.reshape(bsz, n, GLA_V_WIDTH) * norm_g.astype(jnp.float32)
    return (on * jax.nn.silu(og.astype(jnp.float32))).astype(og.dtype) @ w_proj


def _token_mixer(u_ctx, u_lat, rows, need_ctx, w_in, s5_dirs, s5_d, s5_w_glu, rw_dirs, rw_g_up,
                 rw_ln_w, rw_ln_b, rw_w_proj, gl_dirs, gl_norm_g, gl_w_proj, w_out):
    points = _split_points()
    cc = jnp.split(u_ctx @ w_in, points, axis=-1)
    cl = jnp.split(u_lat @ w_in, points, axis=-1)
    ya_ctx, ya_lat = _bidirectional(_s5_direction, s5_dirs[0], s5_dirs[1], (cc[0],), (cl[0],))
    yb_ctx, yb_lat = _bidirectional(_rwkv_direction, rw_dirs[0], rw_dirs[1], (cc[1],), (cl[1],))
    lat_cm = tuple(_to_column_major(t, rows) for t in cl[3:7])
    yc_ctx, yc_lat = _bidirectional(_gla_direction, gl_dirs[0], gl_dirs[1], tuple(cc[3:7]), lat_cm)
    yc_lat = _to_row_major(yc_lat, rows)

    def merge(cols, ya, yb, yc):
        gate_a, gate_b, gate_c = jnp.split(jax.nn.sigmoid(cols[8]), N_BRANCH, axis=-1)
        pa = _s5_out(ya, cols[0], s5_d, s5_w_glu)
        pb = _rwkv_out(yb, cols[2], rw_g_up, rw_ln_w, rw_ln_b, rw_w_proj)
        pc = _gla_out(yc, cols[7], gl_norm_g, gl_w_proj)
        return (gate_a * pa + gate_b * pb + gate_c * pc) @ w_out

    y_lat = merge(cl, ya_lat, yb_lat, yc_lat)
    y_ctx = merge(cc, ya_ctx, yb_ctx, yc_ctx) if need_ctx else None
    return y_ctx, y_lat


def _moe(v, wg1, bg1, wg2, bg2, w_gate, w_up, w_down):
    lead = v.shape[:-1]
    t = v.reshape(-1, v.shape[-1])
    p_group = jax.nn.softmax((t @ wg1).astype(jnp.float32) + bg1.astype(jnp.float32), axis=-1)
    p_top, grp = lax.top_k(p_group, 1)
    logits = ((t @ wg2).astype(jnp.float32) + bg2.astype(jnp.float32)).reshape(-1, MOE_GROUPS, MOE_PER_GROUP)
    in_group = jnp.take_along_axis(logits, grp[:, :, None], axis=1)[:, 0]
    val, idx = lax.top_k(in_group, MOE_TOPK)
    weight = p_top * jax.nn.softmax(val, axis=-1)
    expert = grp * MOE_PER_GROUP + idx
    comb = jnp.einsum('tk,tke->te', weight, jax.nn.one_hot(expert, MOE_EXPERTS, dtype=jnp.float32)).astype(t.dtype)
    hid = jax.nn.silu(jnp.einsum('td,edf->tef', t, w_gate)) * jnp.einsum('td,edf->tef', t, w_up)
    out = jnp.einsum('tef,efd->td', hid * comb[:, :, None], w_down)
    return out.reshape(*lead, out.shape[-1])


def setup_inputs(seed: int = 0) -> dict:
    key = jax.random.key(seed)
    ks = iter(jax.random.split(key, 64))

    def nrm(shape, scale=1.0):
        return jax.random.normal(next(ks), shape, jnp.float32) * scale

    L, D = DEPTH, D_MODEL
    G, P, I = S5_GROUPS, S5_STATE, S5_GROUP
    n_idx = jnp.arange(P, dtype=jnp.float32)
    return {
        'x': nrm((BATCH, SEQ, D)),
        'c': nrm((BATCH, D)),
        'ctx': nrm((BATCH, CTX_LEN, D)),
        'c_ctx': nrm((D,)),
        'w_mod': nrm((L, D, 6 * D), 0.5 * D ** -0.5),
        'b_mod': nrm((L, 6 * D), 0.02),
        'g_norm1': 1.0 + nrm((L, D), 0.05),
        'g_norm2': 1.0 + nrm((L, D), 0.05),
        'w_in': nrm((L, D, IN_WIDTH), D ** -0.5),
        's5_a_re': -0.5 + nrm((L, 2, G, P), 0.01),
        's5_a_im': math.pi * n_idx + nrm((L, 2, G, P), 0.01),
        's5_log_dt': jax.random.uniform(next(ks), (L, 2, G), jnp.float32,
                                        math.log(S5_DT_MIN), math.log(S5_DT_MAX)),
        's5_b_re': nrm((L, 2, G, P, I), (2 * I) ** -0.5),
        's5_b_im': nrm((L, 2, G, P, I), (2 * I) ** -0.5),
        's5_c_re': nrm((L, 2, G, I, P), (2 * P) ** -0.5),
        's5_c_im': nrm((L, 2, G, I, P), (2 * P) ** -0.5),
        's5_d': nrm((L, S5_WIDTH)),
        's5_w_glu': nrm((L, S5_WIDTH, 2 * D), S5_WIDTH ** -0.5),
        'rw_mu': jax.random.uniform(next(ks), (L, 2, RW_SHIFT_WIDTH), jnp.float32),
        'rw_w0': nrm((L, 2, RW_WIDTH), 0.5),
        'rw_w_up': nrm((L, 2, RW_DECAY_RANK, RW_WIDTH), 0.1),
        'rw_a0': nrm((L, 2, RW_WIDTH), 0.5),
        'rw_a_up': nrm((L, 2, RW_ICLR_RANK, RW_WIDTH), 0.1),
        'rw_k_k': 0.85 + nrm((L, RW_WIDTH), 0.05),
        'rw_k_a': 1.0 + nrm((L, RW_WIDTH), 0.05),
        'rw_r_k': nrm((L, RW_HEADS, RW_HEAD), 0.1),
        'rw_g_up': nrm((L, RW_GATE_RANK, RW_WIDTH), RW_GATE_RANK ** -0.5),
        'rw_ln_w': 1.0 + nrm((L, RW_WIDTH), 0.05),
        'rw_ln_b': nrm((L, RW_WIDTH), 0.02),
        'rw_w_proj': nrm((L, RW_WIDTH, D), RW_WIDTH ** -0.5),
        'gl_gk_up': nrm((L, 2, GLA_GATE_RANK, GLA_QK_WIDTH), GLA_GATE_RANK ** -0.5),
        'gl_gk_b': nrm((L, 2, GLA_QK_WIDTH), 0.5),
        'gl_norm_g': 1.0 + nrm((L, GLA_V_WIDTH), 0.05),
        'gl_w_proj': nrm((L, GLA_V_WIDTH, D), GLA_V_WIDTH ** -0.5),
        'w_out': nrm((L, D, D), D ** -0.5),
        'moe_wg1': nrm((L, D, MOE_GROUPS), D ** -0.5),
        'moe_bg1': nrm((L, MOE_GROUPS), 0.01),
        'moe_wg2': nrm((L, D, MOE_EXPERTS), D ** -0.5),
        'moe_bg2': nrm((L, MOE_EXPERTS), 0.01),
        'moe_w_gate': nrm((L, MOE_EXPERTS, D, MOE_HIDDEN), D ** -0.5),
        'moe_w_up': nrm((L, MOE_EXPERTS, D, MOE_HIDDEN), D ** -0.5),
        'moe_w_down': nrm((L, MOE_EXPERTS, MOE_HIDDEN, D), MOE_HIDDEN ** -0.5),
        'g_final': 1.0 + nrm((D,), 0.05),
    }


def reference(x, c, ctx, c_ctx, w_mod, b_mod, g_norm1, g_norm2, w_in,
              s5_a_re, s5_a_im, s5_log_dt, s5_b_re, s5_b_im, s5_c_re, s5_c_im, s5_d, s5_w_glu,
              rw_mu, rw_w0, rw_w_up, rw_a0, rw_a_up, rw_k_k, rw_k_a, rw_r_k, rw_g_up,
              rw_ln_w, rw_ln_b, rw_w_proj, gl_gk_up, gl_gk_b, gl_norm_g, gl_w_proj, w_out,
              moe_wg1, moe_bg1, moe_wg2, moe_bg2, moe_w_gate, moe_w_up, moe_w_down, g_final):
    rows = x.shape[1] // GRID_W
    silu_c = jax.nn.silu(c)
    silu_cc = jax.nn.silu(c_ctx)
    for i in range(DEPTH):
        need_ctx = i < DEPTH - 1
        mod_lat = jnp.split((silu_c @ w_mod[i] + b_mod[i])[:, None, :], 6, axis=-1)
        mod_ctx = jnp.split(silu_cc @ w_mod[i] + b_mod[i], 6, axis=-1)
        u_lat = _modulate(_rmsnorm(x, g_norm1[i]), mod_lat[0], mod_lat[1])
        u_ctx = _modulate(_rmsnorm(ctx, g_norm1[i]), mod_ctx[0], mod_ctx[1])
        s5_dirs = [tuple(p[i, d] for p in (s5_a_re, s5_a_im, s5_log_dt, s5_b_re, s5_b_im, s5_c_re, s5_c_im))
                   for d in range(2)]
        rw_dirs = [(rw_mu[i, d], rw_w0[i, d], rw_w_up[i, d], rw_a0[i, d], rw_a_up[i, d],
                    rw_k_k[i], rw_k_a[i], rw_r_k[i]) for d in range(2)]
        gl_dirs = [(gl_gk_up[i, d], gl_gk_b[i, d]) for d in range(2)]
        y_ctx, y_lat = _token_mixer(u_ctx, u_lat, rows, need_ctx, w_in[i], s5_dirs, s5_d[i], s5_w_glu[i],
                                    rw_dirs, rw_g_up[i], rw_ln_w[i], rw_ln_b[i], rw_w_proj[i],
                                    gl_dirs, gl_norm_g[i], gl_w_proj[i], w_out[i])
        moe_p = (moe_wg1[i], moe_bg1[i], moe_wg2[i], moe_bg2[i], moe_w_gate[i], moe_w_up[i], moe_w_down[i])
        x = x + mod_lat[2] * y_lat
        v_lat = _modulate(_rmsnorm(x, g_norm2[i]), mod_lat[3], mod_lat[4])
        x = x + mod_lat[5] * _moe(v_lat, *moe_p)
        if need_ctx:
            ctx = ctx + mod_ctx[2] * y_ctx
            v_ctx = _modulate(_rmsnorm(ctx, g_norm2[i]), mod_ctx[3], mod_ctx[4])
            ctx = ctx + mod_ctx[5] * _moe(v_ctx, *moe_p)
    return _rmsnorm(x, g_final)
```

```python
import numpy as np
import concourse.bass as bass
import concourse.mybir as mybir

F32 = mybir.dt.float32
BF16 = mybir.dt.bfloat16
I32 = mybir.dt.int32
ALU = mybir.AluOpType
AF = mybir.ActivationFunctionType
AX = mybir.AxisListType
ND = 6


class Buf:
    __slots__ = ("w", "r")

    def __init__(self):
        self.w = None
        self.r = {}


class T:
    def __init__(self, t, buf=None):
        self.t = t
        self.buf = buf or Buf()

    def __getitem__(self, idx):
        return V(self, self.t[idx])

    def ap(self):
        return self.t[:] if not isinstance(self.t, bass.AP) else self.t


class V:
    def __init__(self, tile, ap):
        self.tile = tile
        self.a = ap

    @property
    def buf(self):
        return self.tile.buf

    def ap(self):
        return self.a

    def __getitem__(self, idx):
        return V(self.tile, self.a[idx])


def _ap(x):
    if isinstance(x, (T, V)):
        return x.ap()
    return x


def _bufs(xs):
    out = []
    for x in xs:
        if isinstance(x, (T, V)):
            out.append(x.buf)
        elif isinstance(x, Buf):
            out.append(x)
    return out


class Prog:
    def __init__(self, nc, stack, same_engine_sync=True):
        self.nc = nc
        self.stack = stack
        self.stack_root = stack
        self.same = same_engine_sync
        self.eng = {"pe": nc.tensor, "dve": nc.vector, "act": nc.scalar, "pool": nc.gpsimd, "sp": nc.sync}
        self.esem = {e: stack.enter_context(nc.semaphore(f"s_{e}")) for e in ["pe", "dve", "act", "pool"]}
        self.ecnt = {e: 0 for e in self.esem}
        self.dq = ["sp", "act", "pool"]
        self.dsem = {q: [stack.enter_context(nc.semaphore(f"d_{q}{i}")) for i in range(ND)] for q in self.dq}
        self.dcnt = {q: [0] * ND for q in self.dq}
        self.dnext = {q: 0 for q in self.dq}
        self.ops = {e: [] for e in self.eng}
        self.seen = {e: {} for e in self.eng}
        self.semname = {}
        self.n = 0
        self.nt = 0

    def tile(self, shape, dtype=F32, name=None):
        self.nt += 1
        name = name or f"t{self.nt}"
        return T(self.stack.enter_context(self.nc.sbuf_tensor(name, list(shape), dtype)))

    def psum(self, shape=(128, 512), dtype=F32, name=None):
        self.nt += 1
        name = name or f"p{self.nt}"
        return T(self.stack.enter_context(self.nc.psum_tensor(name, list(shape), dtype)))

    def dram(self, name, shape, dtype=F32, kind="Internal"):
        return T(self.nc.dram_tensor(name, list(shape), dtype, kind=kind).ap())

    def _deps(self, e, reads, writes, skip_sem=None):
        waits = {}

        def need(sem, val):
            if sem is skip_sem:
                return
            k = id(sem)
            if self.seen[e].get(k, 0) >= val:
                return
            if k not in waits or waits[k][1] < val:
                waits[k] = (sem, val)

        for b in reads:
            if b.w is not None:
                need(*b.w)
        for b in writes:
            if b.w is not None:
                need(*b.w)
            for sem, val in b.r.values():
                need(sem, val)
        for k, (sem, val) in waits.items():
            self.seen[e][k] = val
        return list(waits.values())

    def _commit(self, reads, writes, dep):
        sem, val = dep
        for b in reads:
            b.r[id(sem)] = (sem, val)
        for b in writes:
            b.w = dep
            b.r = {}

    def op(self, e, fn, reads=(), writes=()):
        reads = _bufs(reads)
        writes = _bufs(writes)
        sem = self.esem[e]
        skip = sem if (e == "pe" or not self.same) else None
        waits = self._deps(e, reads, writes, skip)
        self.ecnt[e] += 1
        val = self.ecnt[e]
        self.ops[e].append((waits, fn, sem, 1))
        self._commit(reads, writes, (sem, val))
        self.n += 1

    def dma(self, out, in_, q="sp", **kw):
        reads = _bufs([in_])
        writes = _bufs([out])
        i = self.dnext[q]
        self.dnext[q] = (i + 1) % ND
        sem = self.dsem[q][i]
        waits = self._deps(q, reads, writes)
        prev = self.dcnt[q][i]
        k = id(sem)
        if prev > 0 and self.seen[q].get(k, 0) < prev:
            waits = [w for w in waits if w[0] is not sem] + [(sem, prev)]
            self.seen[q][k] = prev
        self.dcnt[q][i] += 16
        o, a = _ap(out), _ap(in_)
        self.ops[q].append((waits, lambda eng: eng.dma_start(out=o, in_=a, **kw), sem, 16))
        self._commit(reads, writes, (sem, self.dcnt[q][i]))
        self.n += 1

    def mm(self, out, lhsT, rhs, start=True, stop=True):
        o, l, r = _ap(out), _ap(lhsT), _ap(rhs)
        self.op("pe", lambda eng: eng.matmul(o, l, r, start=start, stop=stop), [lhsT, rhs], [out])

    def tr(self, out, in_, ident):
        o, i, d = _ap(out), _ap(in_), _ap(ident)
        self.op("pe", lambda eng: eng.transpose(o, i, d), [in_, ident], [out])

    def act(self, out, in_, func, bias=None, scale=None, accum_out=None, e="act"):
        o, i = _ap(out), _ap(in_)
        kw = {}
        rd = [in_]
        wr = [out]
        if bias is not None:
            kw["bias"] = _ap(bias)
            rd.append(bias)
        if scale is not None:
            kw["scale"] = _ap(scale)
            rd.append(scale)
        if accum_out is not None:
            kw["accum_out"] = _ap(accum_out)
            wr.append(accum_out)
        self.op("act", lambda eng: eng.activation(o, i, func, **kw), rd, wr)

    def tt(self, out, in0, in1, op, e="dve"):
        o, a, b = _ap(out), _ap(in0), _ap(in1)
        self.op(e, lambda eng: eng.tensor_tensor(o, a, b, op), [in0, in1], [out])

    def ts(self, out, in0, s1, op0, s2=None, op1=None, accum_out=None, e="dve"):
        o, a = _ap(out), _ap(in0)
        rd = [in0]
        wr = [out]
        if isinstance(s1, (T, V)):
            rd.append(s1)
        if isinstance(s2, (T, V)):
            rd.append(s2)
        kw = {}
        if op1 is not None:
            kw["op1"] = op1
        if accum_out is not None:
            kw["accum_out"] = _ap(accum_out)
            wr.append(accum_out)
        x1, x2 = _ap(s1), _ap(s2)
        self.op(e, lambda eng: eng.tensor_scalar(o, a, x1, x2, op0, **kw), rd, wr)

    def stt(self, out, in0, scalar, in1, op0, op1):
        o, a, b = _ap(out), _ap(in0), _ap(in1)
        rd = [in0, in1]
        if isinstance(scalar, (T, V)):
            rd.append(scalar)
        s = _ap(scalar)
        self.op("dve", lambda eng: eng.scalar_tensor_tensor(o, a, s, b, op0, op1), rd, [out])

    def copy(self, out, in_, e="dve"):
        o, a = _ap(out), _ap(in_)
        if e == "act":
            self.op("act", lambda eng: eng.copy(o, a), [in_], [out])
        else:
            self.op(e, lambda eng: eng.tensor_copy(o, a), [in_], [out])

    def memset(self, out, val, e="dve"):
        o = _ap(out)
        self.op(e, lambda eng: eng.memset(o, val), [], [out])

    def reduce(self, out, in_, op, axis=AX.X):
        o, a = _ap(out), _ap(in_)
        self.op("dve", lambda eng: eng.tensor_reduce(o, a, axis, op), [in_], [out])

    def recip(self, out, in_):
        o, a = _ap(out), _ap(in_)
        self.op("dve", lambda eng: eng.reciprocal(o, a), [in_], [out])

    def scan(self, out, d0, d1, initial, op0=ALU.mult, op1=ALU.add):
        o, a, b = _ap(out), _ap(d0), _ap(d1)
        rd = [d0, d1]
        if isinstance(initial, (T, V)):
            rd.append(initial)
        ini = _ap(initial)
        self.op("dve", lambda eng: eng.tensor_tensor_scan(o, a, b, ini, op0, op1), rd, [out])

    def finish(self):
        nc = self.nc
        fin = []
        for q in self.dq:
            for i in range(ND):
                if self.dcnt[q][i] > 0:
                    fin.append((self.dsem[q][i], self.dcnt[q][i]))
        for e in self.esem:
            if self.ecnt[e] > 0:
                fin.append((self.esem[e], self.ecnt[e]))
        if getattr(self, 'ccnt', 0) > 0:
            fin.append((self.csem, self.ccnt))
        ops = self.ops

        def replay(lst, eng, final=False):
            for waits, fn, sem, inc in lst:
                for s, v in waits:
                    eng.wait_ge(s, v)
                fn(eng).then_inc(sem, inc)
            if final:
                for s, v in fin:
                    eng.wait_ge(s, v)

        with nc.Block() as block:

            @block.sync
            def _(eng):
                replay(ops["sp"], eng, True)

            @block.tensor
            def _(eng):
                replay(ops["pe"], eng)

            @block.vector
            def _(eng):
                replay(ops["dve"], eng)

            @block.scalar
            def _(eng):
                replay(ops["act"], eng)

            @block.gpsimd
            def _(eng):
                replay(ops["pool"], eng)


def _allgather(self, out, in_, n=8):
    q = "pool"
    reads = _bufs([in_])
    writes = _bufs([out])
    i = self.dnext[q]
    self.dnext[q] = (i + 1) % ND
    sem = self.dsem[q][i]
    waits = self._deps(q, reads, writes)
    prev = self.dcnt[q][i]
    k = id(sem)
    if prev > 0 and self.seen[q].get(k, 0) < prev:
        waits = [w for w in waits if w[0] is not sem] + [(sem, prev)]
        self.seen[q][k] = prev
    self.dcnt[q][i] += 16
    o, a = _ap(out), _ap(in_)
    rg = [list(range(n))]
    self.ops[q].append((waits, lambda eng: eng.collective_compute(
        "AllGather", ALU.bypass, replica_groups=rg, ins=[a], outs=[o]), sem, 16))
    self._commit(reads, writes, (sem, self.dcnt[q][i]))
    self.n += 1


Prog.allgather = _allgather

from contextlib import contextmanager, ExitStack as _ES


def _fence(self):
    fd = {}
    for e, sem in self.esem.items():
        if self.ecnt[e] > 0:
            fd[id(sem)] = (sem, self.ecnt[e])
    for q in self.dq:
        for i in range(ND):
            if self.dcnt[q][i] > 0:
                fd[id(self.dsem[q][i])] = (self.dsem[q][i], self.dcnt[q][i])
    if getattr(self, "ccnt", 0) > 0:
        fd[id(self.csem)] = (self.csem, self.ccnt)
    self.fence_deps = fd


@contextmanager
def _scope(self):
    old = self.stack
    with _ES() as st:
        self.stack = st
        try:
            yield
        finally:
            self.stack = old
            self.fence()


def _tile(self, shape, dtype=F32, name=None):
    self.nt += 1
    name = name or f"t{self.nt}"
    t = T(self.stack.enter_context(self.nc.sbuf_tensor(name, list(shape), dtype)))
    t.buf.r = dict(getattr(self, "fence_deps", {}))
    return t


def _allgather2(self, out, in_, n=8):
    q = "pool"
    if not hasattr(self, "csem"):
        self.csem = self.stack_root.enter_context(self.nc.semaphore("cc_sem"))
        self.ccnt = 0
    reads = _bufs([in_])
    writes = _bufs([out])
    waits = self._deps(q, reads, writes)
    k = id(self.csem)
    if self.ccnt > 0 and self.seen[q].get(k, 0) < self.ccnt:
        waits = [w for w in waits if w[0] is not self.csem] + [(self.csem, self.ccnt)]
        self.seen[q][k] = self.ccnt
    self.ccnt += 1
    o, a = _ap(out).opt(), _ap(in_).opt()
    rg = [list(range(n))]
    self.ops[q].append((waits, lambda eng: eng.collective_compute(
        "AllGather", ALU.bypass, replica_groups=rg, ins=[a], outs=[o]), self.csem, 1))
    self._commit(reads, writes, (self.csem, self.ccnt))
    self.n += 1


Prog.fence = _fence
Prog.scope = _scope
Prog.tile = _tile
Prog.allgather = _allgather2


def _dma_dyn(self, out, in_dep, in_fn, q="sp"):
    reads = _bufs([in_dep])
    writes = _bufs([out])
    i = self.dnext[q]
    self.dnext[q] = (i + 1) % ND
    sem = self.dsem[q][i]
    waits = self._deps(q, reads, writes)
    prev = self.dcnt[q][i]
    k = id(sem)
    if prev > 0 and self.seen[q].get(k, 0) < prev:
        waits = [w for w in waits if w[0] is not sem] + [(sem, prev)]
        self.seen[q][k] = prev
    self.dcnt[q][i] += 16
    o = _ap(out)
    self.ops[q].append((waits, lambda eng: eng.dma_start(out=o, in_=in_fn(self._pid(eng))), sem, 16))
    self._commit(reads, writes, (sem, self.dcnt[q][i]))
    self.n += 1


Prog.dma_dyn = _dma_dyn


def _pid(self, eng):
    c = self.__dict__.setdefault("_pid_cache", {})
    if id(eng) not in c:
        c[id(eng)] = eng.partition_id()
    return c[id(eng)]


Prog._pid = _pid


import numpy as np
from contextlib import ExitStack

D = 2048
KT = 16
NCORE = 8
IN_W = 13584
O_S5, O_RW, O_GLO, O_Q, O_K, O_V, O_GK, O_OG, O_GATE = 0, 1024, 4224, 4352, 4864, 5376, 6400, 6416, 7440
MIXW = 6528
BLK = dict(s5=(0, 'pid'), rw_r=(8, 'pid'), rw_k=(16, 'pid'), rw_v=(24, 'pid'), rw_lo=(32, 'static'), gq=(34, 'pid2'), gk=(38, 'pid2'), gv=(42, 'pid'), ggk=(50, 'static'))


class Cfg:
    def __init__(self, seq=4096, ctx=256, depth=2):
        self.seq, self.ctx, self.depth = seq, ctx, depth
        self.TL, self.TC = seq // 4, ctx // 4
        self.NT = self.TL + self.TC
        self.TG = self.NT * NCORE
        self.blocks = []
        o = 0
        while o < self.NT:
            n = min(512, self.NT - o)
            self.blocks.append((o, n))
            o += n


BIG = ["w_mod", "w_in", "s5_w_glu", "rw_w_proj", "gl_w_proj", "w_out", "moe_w_gate", "moe_w_up", "moe_w_down"]


def big_shape(name, L):
    return {
        "w_mod": (L * D, 6 * D), "w_in": (L * D, IN_W), "s5_w_glu": (L * 1024, 4096),
        "rw_w_proj": (L * 1024, D), "gl_w_proj": (L * 1024, D), "w_out": (L * D, D),
        "moe_w_gate": (L * 32 * D, 256), "moe_w_up": (L * 32 * D, 256), "moe_w_down": (L * 32 * 256, D),
    }[name]


def fm_vec(v):
    sh = v.shape
    n = sh[-1] // 128
    return np.ascontiguousarray(np.swapaxes(v.reshape(*sh[:-1], n, 128), -1, -2))


class Ctx:
    pass


def declare_inputs(nc, cfg, spec):
    return {k: T(nc.dram_tensor(k, list(s), F32, kind="ExternalInput").ap()) for k, s in spec.items()}


class PsumPool:
    def __init__(self, P, n=8):
        self.t = [P.psum() for _ in range(n)]
        self.i = 0

    def get(self):
        t = self.t[self.i]
        self.i = (self.i + 1) % len(self.t)
        return t


class Rot:
    def __init__(self, tiles):
        self.t = tiles
        self.i = 0

    def get(self):
        t = self.t[self.i]
        self.i = (self.i + 1) % len(self.t)
        return t


def linear_fm(P, PS, wrot, w2d, row0, K, col0, ncols, rhs, ntok, evac, q="sp"):
    kt = K // 128
    ci = 0
    c = 0
    while c < ncols:
        cw = min(128, ncols - c)
        wt = wrot.get()
        src = w2d.t[row0:row0 + K, col0 + c:col0 + c + cw].rearrange("(k p) n -> p k n", p=128)
        P.dma(wt[:, 0:kt, 0:cw], V(w2d, src), q=q)
        t0 = 0
        while t0 < ntok:
            n = min(512, ntok - t0)
            ps = PS.get()
            for k in range(kt):
                P.mm(ps[0:cw, 0:n], wt[:, k, 0:cw], rhs[:, k, t0:t0 + n], start=(k == 0), stop=(k == kt - 1))
            evac(ci, cw, t0, n, ps[0:cw, 0:n])
            t0 += n
        c += cw
        ci += 1


def prep_common(inp, cfg):
    L = cfg.depth
    TL, TC = cfg.TL, cfg.TC
    maps = []
    TS = cfg.ctx + cfg.seq
    t = np.arange(TS)
    tcr = np.stack([(t // 64).astype(np.float32), (t % 64).astype(np.float32)])
    for c in range(NCORE):
        b, q = c // 4, c % 4
        m = {}
        m["xin"] = np.concatenate([inp["x"][b, q * TL:(q + 1) * TL], inp["ctx"][b, q * TC:(q + 1) * TC]], 0)
        m["cvT"] = np.ascontiguousarray(np.stack([inp["c"][b], inp["c_ctx"]], 1))
        m["ident"] = np.eye(128, dtype=np.float32)
        m["tcr"] = tcr
        for name in BIG:
            R, C = big_shape(name, L)
            w = inp[name][:L].reshape(R, C)
            m[name + "_sh"] = w[c * (R // 8):(c + 1) * (R // 8)]
        m["b_mod_fm"] = fm_vec(inp["b_mod"][:L])
        m["b_mod"] = inp["b_mod"][:L]
        m["g1_fm"] = fm_vec(inp["g_norm1"][:L])
        m["g2_fm"] = fm_vec(inp["g_norm2"][:L])
        maps.append(m)
    return maps


def phase_weights(P, cx, cfg):
    cx.W = {}
    for name in BIG:
        R, C = big_shape(name, cfg.depth)
        src = P.dram(name + "_src", [R // 8, C])
        full = T(P.nc.dram_tensor(name + "_full", [R, C], F32, addr_space="Shared").ap())
        P.dma(src, cx.inp[name + "_sh"], q="pool")
        P.allgather(full, src)
        cx.W[name] = full


def stage_mod(P, PS, cx, cfg, l):
    mod_fm = cx.mod_fm
    with P.scope():
        scT = P.tile([128, KT, 2])
        P.dma(scT, V(cx.inp["cvT"], cx.inp["cvT"].t.rearrange("(k p) j -> p k j", p=128)))
        P.act(scT, scT, AF.Silu)
        bfm = P.tile([128, 96])
        P.dma(bfm, cx.inp["b_mod_fm"][l])
        wrot = Rot([P.tile([128, KT, 128]) for _ in range(3)])

        def evac(ci, cw, t0, n, ps):
            P.ts(mod_fm[:, ci, :], ps, bfm[:, ci:ci + 1], ALU.add)

        linear_fm(P, PS, wrot, cx.W["w_mod"], l * D, D, 0, 6 * D, scT, 2, evac)
    with P.scope():
        scT = P.tile([128, KT, 2])
        P.dma(scT, V(cx.inp["cvT"], cx.inp["cvT"].t.rearrange("(k p) j -> p k j", p=128)))
        P.act(scT, scT, AF.Silu)
        wr = Rot([P.tile([128, KT, 512]) for _ in range(2)])
        brow = P.tile([2, 4096])
        row = P.tile([2, 4096])
        bm = cx.inp["b_mod"]
        for gi, ch in enumerate((2, 5)):
            P.dma(brow[:, gi * D:(gi + 1) * D], V(bm, bm.t[l, ch * D:(ch + 1) * D].partition_broadcast(2)))
            for cc in range(4):
                wt = wr.get()
                src = cx.W["w_mod"].t[l * D:(l + 1) * D, ch * D + cc * 512: ch * D + (cc + 1) * 512].rearrange("(k p) n -> p k n", p=128)
                P.dma(wt, V(cx.W["w_mod"], src))
                ps = PS.get()
                for k in range(KT):
                    P.mm(ps[0:2, 0:512], scT[:, k, :], wt[:, k, :], start=(k == 0), stop=(k == KT - 1))
                o = gi * D + cc * 512
                P.tt(row[:, o:o + 512], ps[0:2, 0:512], brow[:, o:o + 512], ALU.add)
        P.dma(cx.modrow, row)


def norm_to_fm(P, PS, cx, cfg, xsrc, A, S, jcol_lat, jcol_ctx, u_fm, eps=1e-6):
    TL, TC, NT = cfg.TL, cfg.TC, cfg.NT
    ident = cx.ident
    xr = Rot([P.tile([128, D]) for _ in range(2)])
    st = Rot([P.tile([128, 4]) for _ in range(2)])
    junk = P.tile([128, D])
    t0 = 0
    while t0 < NT:
        n = min(128, NT - t0)
        j = jcol_lat if t0 < TL else jcol_ctx
        xt = xr.get()
        s = st.get()
        P.dma(xt[0:n, :], xsrc[t0:t0 + n, :])
        P.act(junk[0:n, :], xt[0:n, :], AF.Square, accum_out=s[0:n, 0:1])
        P.ts(s[0:n, 1:2], s[0:n, 0:1], 1.0 / D, ALU.mult, eps, ALU.add)
        P.act(s[0:n, 2:3], s[0:n, 1:2], AF.Sqrt)
        P.recip(s[0:n, 3:4], s[0:n, 2:3])
        P.ts(xt[0:n, :], xt[0:n, :], s[0:n, 3:4], ALU.mult)
        for k4 in range(4):
            ps = PS.get()
            for kk in range(4):
                k = k4 * 4 + kk
                P.tr(ps[:, kk * 128:kk * 128 + n], xt[0:n, k * 128:(k + 1) * 128], ident[0:n, 0:n])
            for kk in range(4):
                k = k4 * 4 + kk
                P.ts(u_fm[:, k, t0:t0 + n], ps[:, kk * 128:kk * 128 + n], A[:, k, j:j + 1], ALU.mult, S[:, k, j:j + 1], ALU.add)
        t0 += n


def stage_A(P, PS, cx, cfg, l):
    NT = cfg.NT
    with P.scope():
        g1 = P.tile([128, KT])
        P.dma(g1, cx.inp["g1_fm"][l])
        A = P.tile([128, KT, 2])
        S = P.tile([128, KT, 2])
        P.ts(A, cx.mod_fm[:, 16:32, :], 1.0, ALU.add)
        P.tt(A, A, V(g1, g1.t[:, :].unsqueeze(2).to_broadcast([128, KT, 2])), ALU.mult)
        P.copy(S, cx.mod_fm[:, 0:16, :])
        u_fm = P.tile([128, KT, NT])
        norm_to_fm(P, PS, cx, cfg, cx.xres, A, S, 0, 1, u_fm)
        if "u_fm" in cx.dbg:
            P.dma(cx.dbg["u_fm"], u_fm)
        wrot = Rot([P.tile([128, KT, 128]) for _ in range(3)])
        stg = Rot([P.tile([128, NT]) for _ in range(3)])
        cur = {}

        def evac(ci, cw, t0, n, ps):
            if t0 == 0:
                cur["s"] = stg.get()
            s = cur["s"]
            P.copy(s[0:cw, t0:t0 + n], ps, e="act")
            if t0 + n == NT:
                P.dma(cx.Y1[cur["c0"]:cur["c0"] + cw, :], s[0:cw, :], q="act")
                cur["c0"] += cw

        cur["c0"] = 0
        linear_fm(P, PS, wrot, cx.W["w_in"], l * D, D, 0, IN_W, u_fm, NT, evac)
    P.allgather(cx.G1, cx.Y1[0:MIXW, :])
    stage_select(P, cx, cfg)


def build(cfg, in_spec, stages, dbg_spec=None, nlayers=None):
    nc = bass.Bass("TRN2", target_bir_lowering=False)
    cx = Ctx()
    cx.inp = declare_inputs(nc, cfg, in_spec)
    cx.dbg = {k: T(nc.dram_tensor("dbg_" + k, list(s), F32, kind="ExternalOutput").ap()) for k, s in (dbg_spec or {}).items()}
    out = T(nc.dram_tensor("out", [cfg.TL, D], F32, kind="ExternalOutput").ap())
    NT = cfg.NT
    with ExitStack() as st:
        P = Prog(nc, st)
        cx.P = P
        PS = PsumPool(P, 6)
        cx.ident = P.tile([128, 128], name="ident_sb")
        P.dma(cx.ident, cx.inp["ident"])
        cx.mod_fm = P.tile([128, 96, 2], name="mod_fm_sb")
        cx.xres = P.dram("xres", [NT, D])
        cx.modrow = P.dram("modrow", [2, 4096])
        cx.Y1 = P.dram("Y1", [IN_W, NT])
        cx.G1 = T(nc.dram_tensor("G1", [NCORE * MIXW, NT], F32, addr_space="Shared").ap())
        cx.MyIn = P.dram("MyIn", [7, 128, NCORE, NT])
        cx.Z = P.dram("Z", [384, NCORE * NT])
        cx.G2 = T(nc.dram_tensor("G2", [NCORE * 384, NCORE * NT], F32, addr_space="Shared").ap())
        cx.MyZ = P.dram("MyZ", [NCORE * 384, NT])
        P.dma(cx.xres, cx.inp["xin"])
        phase_weights(P, cx, cfg)
        L = nlayers or cfg.depth
        for l in range(L):
            if "mod" in stages:
                stage_mod(P, PS, cx, cfg, l)
            if "A" in stages:
                stage_A(P, PS, cx, cfg, l)
            if "s5" in stages:
                stage_s5(P, PS, cx, cfg, l)
            if "rw" in stages:
                stage_rw(P, PS, cx, cfg, l)
            if "gla" in stages:
                stage_gla(P, PS, cx, cfg, l)
            if "C" in stages:
                stage_C(P, PS, cx, cfg, l, last=(l == cfg.depth - 1))
        if "final" in stages:
            stage_final(P, PS, cx, cfg, out)
        for k, fn in getattr(cx, "dbg_fns", {}).items():
            fn()
        if "mod_fm" in cx.dbg:
            P.dma(cx.dbg["mod_fm"], cx.mod_fm)
        if "modrow" in cx.dbg:
            P.dma(cx.dbg["modrow"], cx.modrow)
        if "Y1" in cx.dbg:
            P.dma(cx.dbg["Y1"], cx.Y1)
        if "Z" in cx.dbg:
            P.dma(cx.dbg["Z"], cx.Z)
        if "xres" in cx.dbg:
            P.dma(cx.dbg["xres"], cx.xres)
        print("n ops", P.n)
        P.finish()
    return nc


GLA_SCALE = float(128 ** -0.5)
TWO_PI = float(2 * np.pi)
PI = float(np.pi)


def range_reduce(P, x, ki, kf):
    P.ts(ki, x, 1.0 / TWO_PI, ALU.mult)
    P.copy(kf, ki)
    P.stt(x, kf, -TWO_PI, x, ALU.mult, ALU.add)
    P.ts(kf, x, PI, ALU.is_gt, -TWO_PI, ALU.mult)
    P.tt(x, x, kf, ALU.add)
    P.ts(kf, x, -PI, ALU.is_lt, TWO_PI, ALU.mult)
    P.tt(x, x, kf, ALU.add)


def prep_s5(inp, cfg, maps):
    L = cfg.depth
    for c in range(NCORE):
        prm = np.zeros((128, L, 2, 3, 4), np.float32)
        B = np.zeros((L, 2, 2, 4, 128, 128), np.float32)
        C = np.zeros((L, 2, 2, 4, 128, 128), np.float32)
        for pr in range(4):
            for gi in range(2):
                g = 8 * c + 2 * pr + gi
                gl = 2 * pr + gi
                for l in range(L):
                    for d in range(2):
                        prm[64 * gi:64 * gi + 64, l, d, 0, pr] = inp["s5_a_re"][l, d, g]
                        prm[64 * gi:64 * gi + 64, l, d, 1, pr] = inp["s5_a_im"][l, d, g]
                        prm[64 * gi:64 * gi + 64, l, d, 2, pr] = inp["s5_log_dt"][l, d, g]
                        B[l, d, 0, pr, 16 * gl:16 * gl + 16, 64 * gi:64 * gi + 64] = inp["s5_b_re"][l, d, g].T
                        B[l, d, 1, pr, 16 * gl:16 * gl + 16, 64 * gi:64 * gi + 64] = inp["s5_b_im"][l, d, g].T
                        C[l, d, 0, pr, 64 * gi:64 * gi + 64, 16 * gl:16 * gl + 16] = inp["s5_c_re"][l, d, g].T
                        C[l, d, 1, pr, 64 * gi:64 * gi + 64, 16 * gl:16 * gl + 16] = inp["s5_c_im"][l, d, g].T
        maps[c]["s5_prm"] = prm
        maps[c]["s5_B"] = B
        maps[c]["s5_C"] = C
        maps[c]["s5_d_fm"] = np.ascontiguousarray(inp["s5_d"][:L, 128 * c:128 * c + 128].T)


def chain_segments(cfg, maxn=1024):
    segs = [(0, cfg.ctx)]
    o = 0
    while o < cfg.seq:
        n = min(maxn, cfg.seq - o)
        segs.append((cfg.ctx + o, n))
        o += n
    return segs


def chain_view(t, cfg, d, t0, n):
    CTX, TS = cfg.ctx, cfg.ctx + cfg.seq
    if d == 0:
        return t[:, t0:t0 + n]
    if t0 < CTX:
        hi = CTX - 1 - t0
    else:
        hi = TS - 1 - (t0 - CTX)
    lo = hi - n
    if lo < 0:
        return t[:, hi::-1]
    return t[:, hi:lo:-1]


MYKEY = dict(s5=0, rw_r=1, rw_k=2, rw_v=3, gv=4, gq=5, gk=6)


def stage_select(P, cx, cfg):
    g = cx.G1.t.rearrange("(s k p) n -> k p s n", s=NCORE, p=128)
    mi = cx.MyIn
    for key, idx in MYKEY.items():
        blk0, kind = BLK[key]
        if kind == "pid":
            P.dma_dyn(V(mi, mi.t[idx]), cx.G1, lambda pid, blk0=blk0: g[blk0:blk0 + 8][bass.ds(pid, 1)].rearrange("o p s n -> (o p) s n"))
        else:
            P.dma_dyn(V(mi, mi.t[idx]), cx.G1, lambda pid, blk0=blk0: g[blk0:blk0 + 4][bass.ds(pid // 2, 1)].rearrange("o p s n -> (o p) s n"))


def g1_rows(cx, key):
    if key in MYKEY:
        return cx.MyIn, cx.MyIn.t[MYKEY[key]]
    blk0, kind = BLK[key]
    g = cx.G1.t.rearrange("(s k p) n -> k p s n", s=NCORE, p=128)
    return cx.G1, g[blk0]


def load_seq_fm(P, cx, cfg, dst, b, key, nrows=128):
    TL, TC, NT, CTX = cfg.TL, cfg.TC, cfg.NT, cfg.ctx
    tt_, ap_ = g1_rows(cx, key)
    P.dma(V(dst, dst.t[0:nrows, CTX:CTX + 4 * TL].rearrange("p (q t) -> p q t", q=4)), V(tt_, ap_[0:nrows, 4 * b:4 * b + 4, 0:TL]))
    P.dma(V(dst, dst.t[0:nrows, 0:CTX].rearrange("p (q t) -> p q t", q=4)), V(tt_, ap_[0:nrows, 4 * b:4 * b + 4, TL:NT]))


def store_seq_fm(P, cx, cfg, src, b, zrow0, nrows=128, q="sp"):
    TL, TC, NT, CTX = cfg.TL, cfg.TC, cfg.NT, cfg.ctx
    z = cx.Z.t.rearrange("r (s n) -> r s n", s=NCORE)
    P.dma(V(cx.Z, z[zrow0:zrow0 + nrows, 4 * b:4 * b + 4, 0:TL]), V(src, src.t[0:nrows, CTX:CTX + 4 * TL].rearrange("p (q t) -> p q t", q=4)), q=q)
    P.dma(V(cx.Z, z[zrow0:zrow0 + nrows, 4 * b:4 * b + 4, TL:NT]), V(src, src.t[0:nrows, 0:CTX].rearrange("p (q t) -> p q t", q=4)), q=q)


def stage_s5(P, PS, cx, cfg, l):
    CTX, TS = cfg.ctx, cfg.ctx + cfg.seq
    segs = chain_segments(cfg, 1024)
    CH = 1024
    with P.scope():
        U = [P.tile([128, TS]) for _ in range(2)]
        Y = [P.tile([128, TS]) for _ in range(2)]
        for b in range(2):
            load_seq_fm(P, cx, cfg, U[b], b, 's5')
        tcr = P.tile([128, 2, TS])
        P.dma(tcr, V(cx.inp["tcr"], cx.inp["tcr"].t.partition_broadcast(128)))
        dsk = P.tile([128, cfg.depth])
        P.dma(dsk, cx.inp["s5_d_fm"])
        Bt = P.tile([128, 2, 4, 128])
        Ct = P.tile([128, 2, 4, 128])
        prm = P.tile([128, 3, 4])
        sm = P.tile([128, 16, 4])
        smi = P.tile([128, 4], I32)
        smf = P.tile([128, 4])
        ang = P.tile([128, CH])
        ki = P.tile([128, CH], I32)
        kf = P.tile([128, CH])
        sinT = P.tile([128, CH])
        cosT = P.tile([128, CH])
        bure = P.tile([128, CH])
        buim = P.tile([128, CH])
        t1 = P.tile([128, CH])
        t2 = P.tile([128, CH])
        wre = P.tile([128, CH])
        wim = P.tile([128, CH])
        carry = P.tile([128, 2, 2])
        first = True
        for d in range(2):
            P.dma(Bt, V(cx.inp["s5_B"], cx.inp["s5_B"].t[l, d].rearrange("r p c m -> c r p m")))
            P.dma(Ct, V(cx.inp["s5_C"], cx.inp["s5_C"].t[l, d].rearrange("r p m c -> m r p c")))
            P.dma(prm, cx.inp["s5_prm"][:, l, d])
            RE, DT, RHO, TH, PSI, COS, SIN, LRE, LIM, CRE, CIM, NCIM, DEN, TA, TB = range(15)
            s = lambda i: sm[:, i, :]
            P.ts(s(RE), prm[:, 0, :], -1e-4, ALU.min)
            P.act(s(DT), prm[:, 2, :], AF.Exp)
            P.tt(s(TA), s(RE), s(DT), ALU.mult)
            P.act(s(RHO), s(TA), AF.Exp)
            P.tt(s(TH), prm[:, 1, :], s(DT), ALU.mult)
            P.ts(s(PSI), s(TH), 64.0, ALU.mult)
            range_reduce(P, s(PSI), smi, smf)
            P.copy(s(TA), s(TH))
            range_reduce(P, s(TA), smi, smf)
            P.act(s(SIN), s(TA), AF.Sin)
            P.ts(s(TA), s(TH), PI / 2, ALU.add)
            range_reduce(P, s(TA), smi, smf)
            P.act(s(COS), s(TA), AF.Sin)
            P.tt(s(LRE), s(RHO), s(COS), ALU.mult)
            P.ts(s(LRE), s(LRE), -1.0, ALU.add)
            P.tt(s(LIM), s(RHO), s(SIN), ALU.mult)
            P.tt(s(DEN), s(RE), s(RE), ALU.mult)
            P.tt(s(TA), prm[:, 1, :], prm[:, 1, :], ALU.mult)
            P.tt(s(DEN), s(DEN), s(TA), ALU.add)
            P.recip(s(DEN), s(DEN))
            P.tt(s(TA), s(LRE), s(RE), ALU.mult)
            P.tt(s(TB), s(LIM), prm[:, 1, :], ALU.mult)
            P.tt(s(TA), s(TA), s(TB), ALU.add)
            P.tt(s(CRE), s(TA), s(DEN), ALU.mult)
            P.tt(s(TA), s(LIM), s(RE), ALU.mult)
            P.tt(s(TB), s(LRE), prm[:, 1, :], ALU.mult)
            P.tt(s(TA), s(TA), s(TB), ALU.subtract)
            P.tt(s(CIM), s(TA), s(DEN), ALU.mult)
            P.ts(s(NCIM), s(CIM), -1.0, ALU.mult)
            for pr in range(4):
                col = lambda i: sm[:, i, pr:pr + 1]
                for (t0, n) in segs:
                    P.ts(ang[:, 0:n], tcr[:, 0, t0:t0 + n], col(PSI), ALU.mult)
                    P.stt(ang[:, 0:n], tcr[:, 1, t0:t0 + n], col(TH), ang[:, 0:n], ALU.mult, ALU.add)
                    range_reduce(P, ang[:, 0:n], ki[:, 0:n], kf[:, 0:n])
                    P.act(sinT[:, 0:n], ang[:, 0:n], AF.Sin)
                    P.ts(ang[:, 0:n], ang[:, 0:n], PI / 2, ALU.add)
                    P.ts(kf[:, 0:n], ang[:, 0:n], PI, ALU.is_gt, -TWO_PI, ALU.mult)
                    P.tt(ang[:, 0:n], ang[:, 0:n], kf[:, 0:n], ALU.add)
                    P.act(cosT[:, 0:n], ang[:, 0:n], AF.Sin)
                    for b in range(2):
                        uv = chain_view(U[b].t, cfg, d, t0, n)
                        yv = chain_view(Y[b].t, cfg, d, t0, n)
                        o = 0
                        while o < n:
                            m = min(512, n - o)
                            pa, pb = PS.get(), PS.get()
                            P.mm(pa[:, 0:m], Bt[:, 0, pr, :], V(U[b], uv[:, o:o + m]))
                            P.mm(pb[:, 0:m], Bt[:, 1, pr, :], V(U[b], uv[:, o:o + m]))
                            P.ts(t1[:, o:o + m], pb[:, 0:m], col(NCIM), ALU.mult)
                            P.stt(bure[:, o:o + m], pa[:, 0:m], col(CRE), t1[:, o:o + m], ALU.mult, ALU.add)
                            P.ts(t2[:, o:o + m], pa[:, 0:m], col(CIM), ALU.mult)
                            P.stt(buim[:, o:o + m], pb[:, 0:m], col(CRE), t2[:, o:o + m], ALU.mult, ALU.add)
                            o += m
                        P.tt(t1[:, 0:n], cosT[:, 0:n], bure[:, 0:n], ALU.mult)
                        P.tt(t2[:, 0:n], sinT[:, 0:n], buim[:, 0:n], ALU.mult)
                        P.tt(wre[:, 0:n], t1[:, 0:n], t2[:, 0:n], ALU.add)
                        P.tt(t1[:, 0:n], cosT[:, 0:n], buim[:, 0:n], ALU.mult)
                        P.tt(t2[:, 0:n], sinT[:, 0:n], bure[:, 0:n], ALU.mult)
                        P.tt(wim[:, 0:n], t1[:, 0:n], t2[:, 0:n], ALU.subtract)
                        rho_b = V(sm, sm.t[:, RHO, pr:pr + 1].to_broadcast([128, n]))
                        ini_re = 0.0 if t0 == 0 else carry[:, b, 0:1]
                        ini_im = 0.0 if t0 == 0 else carry[:, b, 1:2]
                        P.scan(bure[:, 0:n], rho_b, wre[:, 0:n], ini_re)
                        P.scan(buim[:, 0:n], rho_b, wim[:, 0:n], ini_im)
                        P.copy(carry[:, b, 0:1], bure[:, n - 1:n])
                        P.copy(carry[:, b, 1:2], buim[:, n - 1:n])
                        P.tt(t1[:, 0:n], cosT[:, 0:n], bure[:, 0:n], ALU.mult)
                        P.tt(t2[:, 0:n], sinT[:, 0:n], buim[:, 0:n], ALU.mult)
                        P.tt(wre[:, 0:n], t1[:, 0:n], t2[:, 0:n], ALU.subtract)
                        P.tt(t1[:, 0:n], cosT[:, 0:n], buim[:, 0:n], ALU.mult)
                        P.tt(t2[:, 0:n], sinT[:, 0:n], bure[:, 0:n], ALU.mult)
                        P.stt(wim[:, 0:n], t1[:, 0:n], -1.0, t2[:, 0:n], ALU.mult, ALU.subtract)
                        o = 0
                        while o < n:
                            m = min(512, n - o)
                            py = PS.get()
                            P.mm(py[:, 0:m], Ct[:, 0, pr, :], wre[:, o:o + m], start=True, stop=False)
                            P.mm(py[:, 0:m], Ct[:, 1, pr, :], wim[:, o:o + m], start=False, stop=True)
                            yvv = V(Y[b], yv[:, o:o + m])
                            if d == 0 and pr == 0:
                                P.copy(yvv, py[:, 0:m], e="act")
                            else:
                                P.tt(yvv, yvv, py[:, 0:m], ALU.add)
                            o += m
        for b in range(2):
            o = 0
            while o < TS:
                n = min(CH, TS - o)
                P.stt(t1[:, 0:n], U[b][:, o:o + n], dsk[:, l:l + 1], Y[b][:, o:o + n], ALU.mult, ALU.add)
                P.tt(t2[:, 0:n], t1[:, 0:n], t1[:, 0:n], ALU.mult)
                P.ts(t2[:, 0:n], t2[:, 0:n], 0.044715, ALU.mult, 1.0, ALU.add)
                P.tt(t2[:, 0:n], t2[:, 0:n], t1[:, 0:n], ALU.mult)
                P.act(t2[:, 0:n], t2[:, 0:n], AF.Sigmoid, scale=1.5957691216057308)
                P.tt(Y[b][:, o:o + n], t1[:, 0:n], t2[:, 0:n], ALU.mult)
                o += n
            store_seq_fm(P, cx, cfg, Y[b], b, 0)


def prep_gla(inp, cfg, maps):
    L = cfg.depth
    tri = np.triu(np.ones((64, 64), np.float32))
    for c in range(NCORE):
        h = c // 2
        maps[c]["gl_up"] = np.ascontiguousarray(inp["gl_gk_up"][:L, :, :, 128 * h:128 * h + 128])
        maps[c]["gl_b"] = np.ascontiguousarray(inp["gl_gk_b"][:L, :, 128 * h:128 * h + 128])
        maps[c]["tri"] = tri


def permute_cm(P, cfg, dst, src, nrows, to_cm=True):
    CTX, SEQ = cfg.ctx, cfg.seq
    rows = SEQ // 64
    P.copy(dst[0:nrows, 0:CTX], src[0:nrows, 0:CTX], e="act")
    if to_cm:
        s = src.t[0:nrows, CTX:CTX + SEQ].rearrange("p (r c) -> p c r", c=64)
        d = dst.t[0:nrows, CTX:CTX + SEQ].rearrange("p (c r) -> p c r", c=64)
    else:
        s = src.t[0:nrows, CTX:CTX + SEQ].rearrange("p (c r) -> p r c", c=64)
        d = dst.t[0:nrows, CTX:CTX + SEQ].rearrange("p (r c) -> p r c", c=64)
    P.copy(V(dst, d), V(src, s))


def stage_gla(P, PS, cx, cfg, l):
    CTX, SEQ, TS = cfg.ctx, cfg.seq, cfg.ctx + cfg.seq
    NCH = TS // 64
    ident = cx.ident
    with P.scope():
        nat = P.tile([128, TS])
        qp, kp, vp, gp, O = (P.tile([128, TS]) for _ in range(5))
        tri = P.tile([64, 64])
        P.dma(tri, cx.inp["tri"])
        gup = P.tile([16, 2, 128])
        P.dma(gup, V(cx.inp["gl_up"], cx.inp["gl_up"].t[l].rearrange("d r n -> r d n")))
        gb = P.tile([64, 2, 128])
        P.dma(gb, V(cx.inp["gl_b"], cx.inp["gl_b"].t[l].partition_broadcast(64)))
        S = [P.tile([128, 128]) for _ in range(2)]
        la = P.tile([64, 128])
        bfm = P.tile([128, 64])
        sc = P.tile([128, 4])
        e1 = P.tile([128, 64])
        qf = P.tile([128, 64])
        kf = P.tile([128, 64])
        qb = P.tile([128, 64])
        kl = P.tile([128, 64])
        sT = P.tile([64, 64])
        vtm = P.tile([64, 128])
        kltm = P.tile([64, 128])
        g = cx.G1.t.rearrange("(s r) n -> r s n", s=NCORE)
        for b in range(2):
            load_seq_fm(P, cx, cfg, nat, b, 'gq')
            permute_cm(P, cfg, qp, nat, 128)
            load_seq_fm(P, cx, cfg, nat, b, 'gk')
            permute_cm(P, cfg, kp, nat, 128)
            load_seq_fm(P, cx, cfg, nat, b, 'gv')
            permute_cm(P, cfg, vp, nat, 128)
            load_seq_fm(P, cx, cfg, nat, b, 'ggk', nrows=16)
            permute_cm(P, cfg, gp, nat, 16)
            for d in range(2):
                Sd = S[d]
                P.memset(Sd, 0.0)
                if d == 1:
                    def rev(src, nrows):
                        nonlocal nat
                        P.copy(nat[0:nrows, 0:CTX], V(src, src.t[0:nrows, CTX - 1::-1]), e="act")
                        P.copy(nat[0:nrows, CTX:TS], V(src, src.t[0:nrows, TS - 1:CTX - 1:-1]))
                        old = nat
                        nat = src
                        return old
                    qp = rev(qp, 128)
                    kp = rev(kp, 128)
                    vp = rev(vp, 128)
                    gp = rev(gp, 16)
                for j in range(NCH):
                    t0 = 64 * j
                    qc = qp[:, t0:t0 + 64]
                    kc = kp[:, t0:t0 + 64]
                    vc = vp[:, t0:t0 + 64]
                    gc = gp[0:16, t0:t0 + 64]
                    ov = V(O, chain_view(O.t, cfg, d, t0, 64))
                    p1 = PS.get()
                    P.mm(p1[0:64, 0:128], gc, gup[:, d, :])
                    P.tt(la, p1[0:64, 0:128], gb[:, d, :], ALU.add)
                    P.act(la, la, AF.Exp, scale=-1.0)
                    P.act(la, la, AF.Ln, bias=1.0)
                    P.ts(la, la, -1.0 / 16.0, ALU.mult)
                    p2 = PS.get()
                    P.mm(p2[:, 0:64], la, tri)
                    P.copy(bfm, p2[:, 0:64], e="act")
                    P.ts(sc[:, 0:1], bfm[:, 32:33], -1.0, ALU.mult)
                    P.act(sc[:, 2:3], bfm[:, 63:64], AF.Exp)
                    P.act(e1, bfm, AF.Exp, bias=sc[:, 0:1])
                    P.stt(qf, qc, GLA_SCALE, e1, ALU.mult, ALU.mult)
                    P.act(e1, bfm, AF.Exp, bias=bfm[:, 32:33], scale=-1.0)
                    P.tt(kf, kc, e1, ALU.mult)
                    P.act(e1, bfm, AF.Exp)
                    P.stt(qb, qc, GLA_SCALE, e1, ALU.mult, ALU.mult)
                    P.act(e1, bfm, AF.Exp, bias=bfm[:, 63:64], scale=-1.0)
                    P.tt(kl, kc, e1, ALU.mult)
                    p3 = PS.get()
                    P.mm(p3[0:64, 0:64], kf, qf)
                    P.tt(sT, p3[0:64, 0:64], tri, ALU.mult)
                    p4 = PS.get()
                    P.tr(p4[0:64, 0:128], vc, ident)
                    P.tr(p4[0:64, 128:256], kl, ident)
                    P.copy(vtm, p4[0:64, 0:128], e="act")
                    P.copy(kltm, p4[0:64, 128:256], e="act")
                    p5 = PS.get()
                    P.mm(p5[:, 0:64], vtm, sT, start=True, stop=False)
                    P.mm(p5[:, 0:64], Sd, qb, start=False, stop=True)
                    if d == 0:
                        P.copy(ov, p5[:, 0:64], e="act")
                    else:
                        P.tt(ov, ov, p5[:, 0:64], ALU.add)
                    p6 = PS.get()
                    P.mm(p6[:, 0:128], kltm, vtm)
                    P.stt(Sd, Sd, sc[:, 2:3], p6[:, 0:128], ALU.mult, ALU.add)
            permute_cm(P, cfg, nat, O, 128, to_cm=False)
            store_seq_fm(P, cx, cfg, nat, b, 256)


RW_DECAY_SCALE = 0.606531


def prep_rw(inp, cfg, maps):
    L = cfg.depth
    blk = np.zeros((128, 128), np.float32)
    blk[:64, :64] = 1
    blk[64:, 64:] = 1
    i2 = np.concatenate([np.eye(64, dtype=np.float32)] * 2, 0)
    for c in range(NCORE):
        sl = slice(128 * c, 128 * c + 128)
        mu = inp["rw_mu"][:L]
        mu4 = np.stack([mu[:, :, 0:1024][:, :, sl], mu[:, :, 1024:2048][:, :, sl], mu[:, :, 2048:3072][:, :, sl], mu[:, :, 3072:3200]], 2)
        maps[c]["rw_mu_fm"] = np.ascontiguousarray(mu4.transpose(3, 0, 1, 2))
        p1 = np.stack([inp["rw_w0"][:L, :, sl], inp["rw_a0"][:L, :, sl]], 2)
        maps[c]["rw_p1_fm"] = np.ascontiguousarray(p1.transpose(3, 0, 1, 2))
        p2 = np.stack([inp["rw_k_k"][:L, sl], inp["rw_k_a"][:L, sl], inp["rw_r_k"][:L].reshape(L, 1024)[:, sl]], 1)
        maps[c]["rw_p2_fm"] = np.ascontiguousarray(p2.transpose(2, 0, 1))
        up = np.concatenate([inp["rw_w_up"][:L, :, :, sl], inp["rw_a_up"][:L, :, :, sl]], 2)
        maps[c]["rw_up"] = np.ascontiguousarray(up)
        maps[c]["blk64"] = blk
        maps[c]["i2"] = i2


def load_nat_range(P, cx, cfg, dst, col0, key, nrows, b, region, a, n):
    TL, TC, NT = cfg.TL, cfg.TC, cfg.NT
    slab = TC if region == "ctx" else TL
    base = TL if region == "ctx" else 0
    o = 0
    while o < n:
        q = (a + o) // slab
        off = (a + o) % slab
        m = min(n - o, slab - off)
        sidx = 4 * b + q
        dv = dst[0:nrows, col0 + o:col0 + o + m]
        tt_, ap_ = g1_rows(cx, key)
        if m == 1:
            P.dma(dv, V(tt_, ap_[0:nrows, sidx, base + off:base + off + m]), allow_slow_non_contiguous=True)
        else:
            P.dma(dv, V(tt_, ap_[0:nrows, sidx, base + off:base + off + m]))
        o += m


def stage_rw(P, PS, cx, cfg, l):
    CTX, SEQ, TS = cfg.ctx, cfg.seq, cfg.ctx + cfg.seq
    TCH = 128
    chunks = [("ctx", t, TCH) for t in range(0, CTX, TCH)] + [("lat", t, TCH) for t in range(0, SEQ, TCH)]
    with P.scope():
        Yacc = [P.tile([128, TS]) for _ in range(2)]
        written = set()
        blk = P.tile([128, 128])
        P.dma(blk, cx.inp["blk64"])
        i2 = P.tile([128, 64])
        P.dma(i2, cx.inp["i2"])
        ones = P.tile([128, 64])
        P.memset(ones, 1.0)
        mu = P.tile([128, 2, 4])
        P.dma(mu, cx.inp["rw_mu_fm"][:, l])
        omu = P.tile([128, 2, 4])
        P.ts(omu, mu, -1.0, ALU.mult, 1.0, ALU.add)
        p1 = P.tile([128, 2, 2])
        P.dma(p1, cx.inp["rw_p1_fm"][:, l])
        p2 = P.tile([128, 4])
        P.dma(p2[:, 0:3], cx.inp["rw_p2_fm"][:, l])
        P.ts(p2[:, 3:4], p2[:, 1:2], -1.0, ALU.mult, 1.0, ALU.add)
        up = P.tile([128, 2, 128])
        P.dma(up, V(cx.inp["rw_up"], cx.inp["rw_up"].t[l].rearrange("d r n -> r d n")))
        ST = P.tile([128, 4, 64])
        P.memset(ST, 0.0)
        T1 = P.tile([128, 4, 64])
        X = P.tile([128, 4, TCH + 1])
        M = P.tile([128, 4, TCH])
        tmp = P.tile([128, TCH])
        tmp2 = P.tile([128, TCH])
        a_t = P.tile([128, TCH])
        kk = P.tile([128, TCH])
        YB = [P.tile([128, TCH]) for _ in range(4)]
        KKc, NKc, Wc, Rc, KPc, Vc = (P.tile([128, TCH, 4]) for _ in range(6))
        Dg = Rot([P.tile([128, 8, 64]) for _ in range(2)])
        KV = Rot([P.tile([128, 8, 4, 64]) for _ in range(2)])
        PSa = P.psum([128, 4, 64])
        PSy = P.psum([128, 4, 128])
        n = TCH
        for ci, (region, c0, _) in enumerate(chunks):
            rlen = CTX if region == "ctx" else SEQ
            rbase = 0 if region == "ctx" else CTX
            for j in range(4):
                b, d = j // 2, j % 2
                a = c0 if d == 0 else rlen - c0 - n
                for fi, rf in enumerate(('rw_r', 'rw_k', 'rw_v')):
                    if d == 0:
                        if a == 0:
                            P.memset(X[:, fi, 0:1], 0.0)
                            load_nat_range(P, cx, cfg, V(X, X.t[:, fi, :]), 1, rf, 128, b, region, a, n)
                        else:
                            load_nat_range(P, cx, cfg, V(X, X.t[:, fi, :]), 0, rf, 128, b, region, a - 1, n + 1)
                    else:
                        if a + n == rlen:
                            P.memset(X[:, fi, n:n + 1], 0.0)
                            load_nat_range(P, cx, cfg, V(X, X.t[:, fi, :]), 0, rf, 128, b, region, a, n)
                        else:
                            load_nat_range(P, cx, cfg, V(X, X.t[:, fi, :]), 0, rf, 128, b, region, a, n + 1)
                fi = 3
                ro = 'rw_lo'
                if d == 0:
                    if a == 0:
                        P.memset(X[:, fi, 0:1], 0.0)
                        load_nat_range(P, cx, cfg, V(X, X.t[:, fi, :]), 1, ro, 128, b, region, a, n)
                    else:
                        load_nat_range(P, cx, cfg, V(X, X.t[:, fi, :]), 0, ro, 128, b, region, a - 1, n + 1)
                else:
                    if a + n == rlen:
                        P.memset(X[:, fi, n:n + 1], 0.0)
                        load_nat_range(P, cx, cfg, V(X, X.t[:, fi, :]), 0, ro, 128, b, region, a, n)
                    else:
                        load_nat_range(P, cx, cfg, V(X, X.t[:, fi, :]), 0, ro, 128, b, region, a, n + 1)
                for fi in range(4):
                    if d == 0:
                        z, pv = X[:, fi, 1:n + 1], X[:, fi, 0:n]
                    else:
                        z, pv = X[:, fi, 0:n], X[:, fi, 1:n + 1]
                    P.ts(tmp, z, omu[:, d, fi:fi + 1], ALU.mult)
                    P.stt(M[:, fi, :], pv, mu[:, d, fi:fi + 1], tmp, ALU.mult, ALU.add)
                r_, k_, v_, lo = M[:, 0, :], M[:, 1, :], M[:, 2, :], M[:, 3, :]
                def cv(tile):
                    return V(tile, tile.t[:, :, j] if d == 0 else tile.t[:, ::-1, j])
                P.act(tmp[0:64, :], M[0:64, 3, :], AF.Tanh)
                pw = PS.get()
                P.mm(pw[:, 0:n], up[0:64, d, :], tmp[0:64, :])
                pw2 = PS.get()
                P.mm(pw2[:, 0:n], up[64:128, d, :], M[64:128, 3, :])
                P.act(tmp2, pw[:, 0:n], AF.Sigmoid, bias=p1[:, d, 0:1])
                P.act(cv(Wc), tmp2, AF.Exp, scale=-RW_DECAY_SCALE)
                P.act(a_t, pw2[:, 0:n], AF.Sigmoid, bias=p1[:, d, 1:2])
                P.ts(kk, k_, p2[:, 0:1], ALU.mult)
                P.tt(tmp, kk, kk, ALU.mult)
                pn = PS.get()
                P.mm(pn[:, 0:n], blk, tmp)
                P.act(tmp, pn[:, 0:n], AF.Sqrt)
                P.ts(tmp, tmp, 1e-12, ALU.max)
                P.recip(tmp, tmp)
                P.tt(kk, kk, tmp, ALU.mult)
                P.copy(cv(KKc), kk)
                P.stt(cv(NKc), kk, -1.0, a_t, ALU.mult, ALU.mult)
                P.ts(tmp, a_t, p2[:, 1:2], ALU.mult, p2[:, 3:4], ALU.add)
                P.tt(tmp, tmp, k_, ALU.mult)
                P.copy(cv(KPc), tmp)
                P.copy(cv(Rc), r_)
                P.copy(cv(Vc), v_)
                P.stt(tmp2, r_, p2[:, 2:3], tmp, ALU.mult, ALU.mult)
                pb = PS.get()
                P.mm(pb[:, 0:n], blk, tmp2)
                P.tt(YB[j], pb[:, 0:n], v_, ALU.mult)
            for i0 in range(0, n, 8):
                kv = KV.get()
                for j in range(4):
                    dg = Dg.get()
                    P.tt(dg, V(i2, i2.t[:, :].unsqueeze(1).to_broadcast([128, 8, 64])),
                         V(Vc, Vc.t[:, i0:i0 + 8, j].unsqueeze(2).to_broadcast([128, 8, 64])), ALU.mult, e="pool")
                    pk = PS.get()
                    for h in range(2):
                        P.mm(V(pk, pk.t[64 * h:64 * h + 64, 0:512]), ones[64 * h:64 * h + 64, :],
                             V(dg, dg.t[64 * h:64 * h + 64].rearrange("p e v -> p (e v)")))
                    P.tt(V(kv, kv.t[:, :, j, :]), V(pk, pk.t[:, 0:512].rearrange("p (e v) -> p e v", e=8)),
                         V(KPc, KPc.t[:, i0:i0 + 8, j].unsqueeze(2).to_broadcast([128, 8, 64])), ALU.mult)
                for e in range(8):
                    i = i0 + e
                    for j in range(4):
                        for h in range(2):
                            hs = slice(64 * h, 64 * h + 64)
                            P.mm(V(PSa, PSa.t[hs, j, :]), V(KKc, KKc.t[hs, i, j:j + 1].to_broadcast([64, 64])), V(ST, ST.t[hs, j, :]))
                    P.tt(T1, PSa, V(NKc, NKc.t[:, i, :].unsqueeze(2).to_broadcast([128, 4, 64])), ALU.mult)
                    P.tt(ST, ST, V(Wc, Wc.t[:, i, :].unsqueeze(2).to_broadcast([128, 4, 64])), ALU.mult)
                    P.tt(ST, ST, T1, ALU.add)
                    P.tt(ST, ST, V(kv, kv.t[:, e, :, :]), ALU.add)
                    for j in range(4):
                        for h in range(2):
                            hs = slice(64 * h, 64 * h + 64)
                            P.mm(V(PSy, PSy.t[hs, j, i:i + 1]), V(ST, ST.t[hs, j, :]), V(Rc, Rc.t[hs, i, j:j + 1]))
            for j in range(4):
                b, d = j // 2, j % 2
                a = c0 if d == 0 else rlen - c0 - n
                yv = Yacc[b][:, rbase + a:rbase + a + n]
                src = V(PSy, PSy.t[:, j, 0:n] if d == 0 else PSy.t[:, j, n - 1::-1])
                key = (b, rbase + a)
                if key not in written:
                    written.add(key)
                    P.tt(yv, src, YB[j], ALU.add)
                else:
                    P.tt(tmp, src, YB[j], ALU.add)
                    P.tt(yv, yv, tmp, ALU.add)
        for b in range(2):
            store_seq_fm(P, cx, cfg, Yacc[b], b, 128)


def prep_C(inp, cfg, maps):
    L = cfg.depth
    for c in range(NCORE):
        m = maps[c]
        m["rw_ln_fm"] = np.ascontiguousarray(np.stack([fm_vec(inp["rw_ln_w"][:L]), fm_vec(inp["rw_ln_b"][:L])], 1))
        m["rw_g_up"] = np.ascontiguousarray(inp["rw_g_up"][:L])
        m["gl_ng_fm"] = fm_vec(inp["gl_norm_g"][:L])
        m["moe_wg"] = np.ascontiguousarray(np.concatenate([inp["moe_wg1"][:L], inp["moe_wg2"][:L]], -1))
        m["moe_bg"] = np.ascontiguousarray(np.concatenate([inp["moe_bg1"][:L], inp["moe_bg2"][:L]], -1))
        m["g_final"] = inp["g_final"][None, :]
        if "blk64" not in m:
            blk = np.zeros((128, 128), np.float32)
            blk[:64, :64] = 1
            blk[64:, 64:] = 1
            m["blk64"] = blk


def tok_blocks(cfg, maxn, last):
    out = []
    for (lo, hi) in ((0, cfg.TL),) + (() if last else ((cfg.TL, cfg.NT),)):
        o = lo
        while o < hi:
            n = min(maxn, hi - o)
            out.append((o, n))
            o += n
    return out


def stage_C(P, PS, cx, cfg, l, last):
    TL, TC, NT = cfg.TL, cfg.TC, cfg.NT
    P.allgather(cx.G2, cx.Z)
    g2 = cx.G2.t.rearrange("r (d n) -> r d n", d=NCORE)
    P.dma_dyn(cx.MyZ, cx.G2, lambda pid: g2[:, bass.ds(pid, 1), :].rearrange("r o n -> r (o n)"))
    myz = cx.MyZ.t.rearrange("(s m p) n -> m p s n", s=NCORE, m=3)
    y1 = cx.Y1.t
    CUT = 99
    with P.scope():
        NB = 256
        blk = P.tile([128, 128])
        P.dma(blk, cx.inp["blk64"])
        blkm = P.tile([128, 128])
        P.ts(blkm, blk, 1.0 / 64.0, ALU.mult)
        ones = P.tile([128, 128])
        P.memset(ones, 1.0 / 256.0)
        lnp = P.tile([128, 2, 8])
        P.dma(lnp, V(cx.inp["rw_ln_fm"], cx.inp["rw_ln_fm"].t[l].rearrange("a p k -> p a k")))
        gng = P.tile([128, 8])
        P.dma(gng, cx.inp["gl_ng_fm"][l])
        gup = P.tile([128, 1024])
        P.dma(gup, cx.inp["rw_g_up"][l])
        grow = P.tile([128, 2, D])
        for j in range(2):
            P.dma(grow[:, j, :], V(cx.modrow, cx.modrow.t[j, 0:D].partition_broadcast(128)))
        zs, yr, og_ = (P.tile([128, 8, NB]) for _ in range(3))
        mg = P.tile([128, KT, NB])
        gt = P.tile([128, KT, NB])
        xb = P.tile([128, 8, NB])
        so = P.tile([128, 8, NB])
        glo = P.tile([128, NB])
        sgt = P.tile([128, NB])
        t1 = P.tile([128, NB])
        t2 = P.tile([128, NB])
        wrot = Rot([P.tile([128, KT, 128]) for _ in range(3)])
        wo = P.tile([128, KT, 256])
        xt = Rot([P.tile([128, D]) for _ in range(2)])
        for (o, n) in tok_blocks(cfg, NB, last):
            jrow = 0 if o < TL else 1
            P.dma(zs[:, :, 0:n], V(cx.MyZ, myz[0, :, :, o:o + n]))
            P.dma(yr[:, :, 0:n], V(cx.MyZ, myz[1, :, :, o:o + n]))
            P.dma(og_[:, :, 0:n], V(cx.MyZ, myz[2, :, :, o:o + n]))
            P.dma(gt[:, :, 0:n], V(cx.Y1, y1[O_GATE:O_GATE + D, o:o + n].rearrange("(k p) n -> p k n", p=128)))
            P.act(gt[:, :, 0:n], gt[:, :, 0:n], AF.Sigmoid)
            for ct in range(KT):
                def ev_gate(ci, cw, t0, nn, ps):
                    P.act(sgt[:, 0:nn], ps, AF.Sigmoid)
                linear_fm(P, PS, wrot, cx.W["s5_w_glu"], l * 1024, 1024, D + ct * 128, 128, zs, n, ev_gate)
                def ev_val(ci, cw, t0, nn, ps, ct=ct):
                    P.tt(t1[:, 0:nn], ps, sgt[:, 0:nn], ALU.mult)
                    P.tt(mg[:, ct, 0:nn], t1[:, 0:nn], gt[:, ct, 0:nn], ALU.mult)
                linear_fm(P, PS, wrot, cx.W["s5_w_glu"], l * 1024, 1024, ct * 128, 128, zs, n, ev_val)
            P.dma(glo[:, 0:n], V(cx.Y1, y1[O_GLO:O_GLO + 128, o:o + n]))
            P.act(glo[:, 0:n], glo[:, 0:n], AF.Sigmoid)
            for k in range(8):
                pm = PS.get()
                P.mm(pm[:, 0:n], blkm, yr[:, k, 0:n])
                P.tt(t1[:, 0:n], yr[:, k, 0:n], pm[:, 0:n], ALU.subtract)
                P.tt(t2[:, 0:n], t1[:, 0:n], t1[:, 0:n], ALU.mult)
                pv = PS.get()
                P.mm(pv[:, 0:n], blkm, t2[:, 0:n])
                P.ts(t2[:, 0:n], pv[:, 0:n], 64e-5, ALU.add)
                P.act(t2[:, 0:n], t2[:, 0:n], AF.Sqrt)
                P.recip(t2[:, 0:n], t2[:, 0:n])
                P.tt(t1[:, 0:n], t1[:, 0:n], t2[:, 0:n], ALU.mult)
                P.ts(t1[:, 0:n], t1[:, 0:n], lnp[:, 0, k:k + 1], ALU.mult, lnp[:, 1, k:k + 1], ALU.add)
                pg = PS.get()
                P.mm(pg[:, 0:n], gup[:, k * 128:(k + 1) * 128], glo[:, 0:n])
                P.tt(xb[:, k, 0:n], t1[:, 0:n], pg[:, 0:n], ALU.mult)
            P.dma(gt[:, :, 0:n], V(cx.Y1, y1[O_GATE + D:O_GATE + 2 * D, o:o + n].rearrange("(k p) n -> p k n", p=128)))
            P.act(gt[:, :, 0:n], gt[:, :, 0:n], AF.Sigmoid)

            def ev_b(ci, cw, t0, nn, ps):
                P.tt(t1[:, 0:nn], ps, gt[:, ci, 0:nn], ALU.mult)
                P.tt(mg[:, ci, 0:nn], mg[:, ci, 0:nn], t1[:, 0:nn], ALU.add)
            linear_fm(P, PS, wrot, cx.W["rw_w_proj"], l * 1024, 1024, 0, D, xb, n, ev_b)
            P.dma(so[:, :, 0:n], V(cx.Y1, y1[O_OG:O_OG + 1024, o:o + n].rearrange("(k p) n -> p k n", p=128)))
            P.act(so[:, :, 0:n], so[:, :, 0:n], AF.Silu)
            for h in range(4):
                pq = PS.get()
                for e in range(2):
                    k = 2 * h + e
                    P.tt(t1[:, 0:n], og_[:, k, 0:n], og_[:, k, 0:n], ALU.mult)
                    P.mm(pq[:, 0:n], ones, t1[:, 0:n], start=(e == 0), stop=(e == 1))
                P.ts(t2[:, 0:n], pq[:, 0:n], 1e-6, ALU.add)
                P.act(t2[:, 0:n], t2[:, 0:n], AF.Sqrt)
                P.recip(t2[:, 0:n], t2[:, 0:n])
                for e in range(2):
                    k = 2 * h + e
                    P.stt(t1[:, 0:n], og_[:, k, 0:n], gng[:, k:k + 1], t2[:, 0:n], ALU.mult, ALU.mult)
                    P.tt(xb[:, k, 0:n], t1[:, 0:n], so[:, k, 0:n], ALU.mult)
            P.dma(gt[:, :, 0:n], V(cx.Y1, y1[O_GATE + 2 * D:O_GATE + 3 * D, o:o + n].rearrange("(k p) n -> p k n", p=128)))
            P.act(gt[:, :, 0:n], gt[:, :, 0:n], AF.Sigmoid)
            linear_fm(P, PS, wrot, cx.W["gl_w_proj"], l * 1024, 1024, 0, D, xb, n, ev_b)
            ntt = (n + 127) // 128
            xts = []
            for tt_ in range(ntt):
                m = min(128, n - tt_ * 128)
                x_ = xt.get()
                P.dma(x_[0:m, :], cx.xres[o + tt_ * 128:o + tt_ * 128 + m, :])
                xts.append((x_, m))
            for cc in range(D // 256):
                P.dma(wo, V(cx.W["w_out"], cx.W["w_out"].t[l * D:(l + 1) * D, cc * 256:(cc + 1) * 256].rearrange("(k p) n -> p k n", p=128)))
                for tt_, (x_, m) in enumerate(xts):
                    ps = PS.get()
                    for k in range(KT):
                        P.mm(ps[0:m, 0:256], mg[:, k, tt_ * 128:tt_ * 128 + m], wo[:, k, :], start=(k == 0), stop=(k == KT - 1))
                    P.tt(t1[0:m, 0:256], ps[0:m, 0:256], grow[0:m, jrow, cc * 256:(cc + 1) * 256], ALU.mult)
                    P.tt(x_[0:m, cc * 256:(cc + 1) * 256], x_[0:m, cc * 256:(cc + 1) * 256], t1[0:m, 0:256], ALU.add)
            for tt_, (x_, m) in enumerate(xts):
                P.dma(cx.xres[o + tt_ * 128:o + tt_ * 128 + m, :], x_[0:m, :])
    if "xmid" in cx.dbg:
        P.dma(cx.dbg["xmid"], cx.xres)
    if CUT == 1:
        return
    stage_moe(P, PS, cx, cfg, l, last)


def stage_moe(P, PS, cx, cfg, l, last):
    TL, TC, NT = cfg.TL, cfg.TC, cfg.NT
    NB = 512
    with P.scope():
        g2 = P.tile([128, KT])
        P.dma(g2, cx.inp["g2_fm"][l])
        A = P.tile([128, KT, 2])
        S = P.tile([128, KT, 2])
        P.ts(A, cx.mod_fm[:, 64:80, :], 1.0, ALU.add)
        P.tt(A, A, V(g2, g2.t[:, :].unsqueeze(2).to_broadcast([128, KT, 2])), ALU.mult)
        P.copy(S, cx.mod_fm[:, 48:64, :])
        grow = P.tile([128, 2, D])
        for j in range(2):
            P.dma(grow[:, j, :], V(cx.modrow, cx.modrow.t[j, D:2 * D].partition_broadcast(128)))
        wg = P.tile([128, KT, 36])
        P.dma(wg, V(cx.inp["moe_wg"], cx.inp["moe_wg"].t[l].rearrange("(k p) e -> p k e", p=128)))
        bg = P.tile([128, 36])
        P.dma(bg, V(cx.inp["moe_bg"], cx.inp["moe_bg"].t[l].partition_broadcast(128)))
        v_fm = P.tile([128, KT, NB])
        acc = [P.tile([128, D]) for _ in range(NB // 128)]
        comb = [P.tile([128, 32]) for _ in range(NB // 128)]
        rt = P.tile([128, 96])
        wgu = Rot([P.tile([128, 2, KT, 256]) for _ in range(2)])
        wd = P.tile([128, 2, D])
        hg = P.tile([128, 2, NB])
        hid = P.tile([128, 2, NB])
        ident = cx.ident
        xr = Rot([P.tile([128, D]) for _ in range(2)])
        st = Rot([P.tile([128, 4]) for _ in range(2)])
        junk = P.tile([128, D])
        wgate, wup, wdn = cx.W["moe_w_gate"], cx.W["moe_w_up"], cx.W["moe_w_down"]
        for (o, n) in tok_blocks(cfg, NB, last):
            jrow = 0 if o < TL else 1
            ntt = (n + 127) // 128
            tiles = [(tt_, min(128, n - tt_ * 128)) for tt_ in range(ntt)]
            for tt_, m in tiles:
                x_ = xr.get()
                s = st.get()
                P.dma(x_[0:m, :], cx.xres[o + tt_ * 128:o + tt_ * 128 + m, :])
                P.act(junk[0:m, :], x_[0:m, :], AF.Square, accum_out=s[0:m, 0:1])
                P.ts(s[0:m, 1:2], s[0:m, 0:1], 1.0 / D, ALU.mult, 1e-6, ALU.add)
                P.act(s[0:m, 2:3], s[0:m, 1:2], AF.Sqrt)
                P.recip(s[0:m, 3:4], s[0:m, 2:3])
                P.ts(x_[0:m, :], x_[0:m, :], s[0:m, 3:4], ALU.mult)
                for k4 in range(4):
                    ps = PS.get()
                    for kk in range(4):
                        k = k4 * 4 + kk
                        P.tr(ps[:, kk * 128:kk * 128 + m], x_[0:m, k * 128:(k + 1) * 128], ident[0:m, 0:m])
                    for kk in range(4):
                        k = k4 * 4 + kk
                        P.ts(v_fm[:, k, tt_ * 128:tt_ * 128 + m], ps[:, kk * 128:kk * 128 + m], A[:, k, jrow:jrow + 1], ALU.mult, S[:, k, jrow:jrow + 1], ALU.add)
            for tt_, m in tiles:
                pl = PS.get()
                for k in range(KT):
                    P.mm(pl[0:m, 0:36], v_fm[:, k, tt_ * 128:tt_ * 128 + m], wg[:, k, :], start=(k == 0), stop=(k == KT - 1))
                lg = rt[0:m, 0:36]
                P.tt(lg, pl[0:m, 0:36], bg[0:m, :], ALU.add)
                c_ = lambda a, b: rt[0:m, a:b]
                M1, SS, V1, V2, W1, W2, EX = 36, 37, 38, 39, 40, 41, 42
                P.reduce(c_(M1, M1 + 1), c_(0, 4), ALU.max)
                P.ts(c_(44, 48), c_(0, 4), c_(M1, M1 + 1), ALU.subtract)
                P.act(c_(44, 48), c_(44, 48), AF.Exp)
                P.reduce(c_(SS, SS + 1), c_(44, 48), ALU.add)
                P.recip(c_(SS, SS + 1), c_(SS, SS + 1))
                P.ts(c_(48, 52), c_(0, 4), c_(M1, M1 + 1), ALU.is_equal)
                tmp32 = rt[0:m, 64:96]
                P.tt(V(rt, rt.t[0:m, 64:96].rearrange("t (g e) -> t g e", g=4)), V(rt, rt.t[0:m, 4:36].rearrange("t (g e) -> t g e", g=4)),
                     V(rt, rt.t[0:m, 48:52].unsqueeze(2).to_broadcast([m, 4, 8])), ALU.mult)
                P.reduce(c_(52, 60), V(rt, rt.t[0:m, 64:96].rearrange("t (g e) -> t e g", g=4)), ALU.add)
                P.reduce(c_(V1, V1 + 1), c_(52, 60), ALU.max)
                P.ts(c_(64, 72), c_(52, 60), c_(V1, V1 + 1), ALU.is_equal)
                P.stt(c_(72, 80), c_(64, 72), -1e30, c_(52, 60), ALU.mult, ALU.add)
                P.reduce(c_(V2, V2 + 1), c_(72, 80), ALU.max)
                P.ts(c_(80, 88), c_(72, 80), c_(V2, V2 + 1), ALU.is_equal)
                P.tt(c_(EX, EX + 1), c_(V2, V2 + 1), c_(V1, V1 + 1), ALU.subtract)
                P.act(c_(EX, EX + 1), c_(EX, EX + 1), AF.Exp)
                P.ts(c_(W1, W1 + 1), c_(EX, EX + 1), 1.0, ALU.add)
                P.recip(c_(W1, W1 + 1), c_(W1, W1 + 1))
                P.tt(c_(W2, W2 + 1), c_(W1, W1 + 1), c_(EX, EX + 1), ALU.mult)
                P.tt(c_(W1, W1 + 1), c_(W1, W1 + 1), c_(SS, SS + 1), ALU.mult)
                P.tt(c_(W2, W2 + 1), c_(W2, W2 + 1), c_(SS, SS + 1), ALU.mult)
                P.ts(c_(64, 72), c_(64, 72), c_(W1, W1 + 1), ALU.mult)
                P.stt(c_(88, 96), c_(80, 88), c_(W2, W2 + 1), c_(64, 72), ALU.mult, ALU.add)
                P.tt(V(comb[tt_], comb[tt_].t[0:m, :].rearrange("t (g e) -> t g e", g=4)),
                     V(rt, rt.t[0:m, 48:52].unsqueeze(2).to_broadcast([m, 4, 8])),
                     V(rt, rt.t[0:m, 88:96].unsqueeze(1).to_broadcast([m, 4, 8])), ALU.mult)
                P.memset(acc[tt_][0:m, :], 0.0)
            if "comb" in cx.dbg and o == 0:
                P.dma(cx.dbg["comb"], comb[0])
            for e in range(32):
                w2 = wgu.get()
                r0 = (l * 32 + e) * D
                P.dma(w2[:, 0], V(wgate, wgate.t[r0:r0 + D, :].rearrange("(k p) f -> p k f", p=128)))
                P.dma(w2[:, 1], V(wup, wup.t[r0:r0 + D, :].rearrange("(k p) f -> p k f", p=128)))
                r1 = (l * 32 + e) * 256
                P.dma(wd, V(wdn, wdn.t[r1:r1 + 256, :].rearrange("(f p) d -> p f d", p=128)))
                for ft in range(2):
                    pg, pu = PS.get(), PS.get()
                    for k in range(KT):
                        P.mm(pg[:, 0:n], w2[:, 0, k, ft * 128:(ft + 1) * 128], v_fm[:, k, 0:n], start=(k == 0), stop=(k == KT - 1))
                    for k in range(KT):
                        P.mm(pu[:, 0:n], w2[:, 1, k, ft * 128:(ft + 1) * 128], v_fm[:, k, 0:n], start=(k == 0), stop=(k == KT - 1))
                    P.act(hg[:, ft, 0:n], pg[:, 0:n], AF.Silu)
                    P.tt(hid[:, ft, 0:n], hg[:, ft, 0:n], pu[:, 0:n], ALU.mult)
                for tt_, m in tiles:
                    for cc in range(4):
                        pd = PS.get()
                        for ft in range(2):
                            P.mm(pd[0:m, 0:512], hid[:, ft, tt_ * 128:tt_ * 128 + m], wd[:, ft, cc * 512:(cc + 1) * 512], start=(ft == 0), stop=(ft == 1))
                        P.stt(acc[tt_][0:m, cc * 512:(cc + 1) * 512], pd[0:m, 0:512], comb[tt_][0:m, e:e + 1], acc[tt_][0:m, cc * 512:(cc + 1) * 512], ALU.mult, ALU.add)
            for tt_, m in tiles:
                x_ = xr.get()
                P.dma(x_[0:m, :], cx.xres[o + tt_ * 128:o + tt_ * 128 + m, :])
                P.tt(acc[tt_][0:m, :], acc[tt_][0:m, :], grow[0:m, jrow, :], ALU.mult)
                P.tt(x_[0:m, :], x_[0:m, :], acc[tt_][0:m, :], ALU.add)
                P.dma(cx.xres[o + tt_ * 128:o + tt_ * 128 + m, :], x_[0:m, :])


def stage_final(P, PS, cx, cfg, out):
    TL = cfg.TL
    with P.scope():
        gf = P.tile([128, D])
        P.dma(gf, V(cx.inp["g_final"], cx.inp["g_final"].t[0].partition_broadcast(128)))
        xr = Rot([P.tile([128, D]) for _ in range(2)])
        st = Rot([P.tile([128, 4]) for _ in range(2)])
        junk = P.tile([128, D])
        for t0 in range(0, TL, 128):
            x_ = xr.get()
            s = st.get()
            P.dma(x_, cx.xres[t0:t0 + 128, :])
            P.act(junk, x_, AF.Square, accum_out=s[:, 0:1])
            P.ts(s[:, 1:2], s[:, 0:1], 1.0 / D, ALU.mult, 1e-6, ALU.add)
            P.act(s[:, 2:3], s[:, 1:2], AF.Sqrt)
            P.recip(s[:, 3:4], s[:, 2:3])
            P.stt(x_, x_, s[:, 3:4], gf, ALU.mult, ALU.mult)
            P.dma(out[t0:t0 + 128, :], x_)


from concourse.bass_utils import run_bass_kernel_spmd

ALL_STAGES = {"mod", "A", "s5", "rw", "gla", "C", "final"}


def kernel(**inputs):
    cfg = Cfg(seq=4096, ctx=256, depth=2)
    inp = {k: np.asarray(v, dtype=np.float32) for k, v in inputs.items()}
    maps = prep_common(inp, cfg)
    prep_s5(inp, cfg, maps)
    prep_gla(inp, cfg, maps)
    prep_rw(inp, cfg, maps)
    prep_C(inp, cfg, maps)
    maps = [{k: np.ascontiguousarray(v, dtype=np.float32) for k, v in m.items()} for m in maps]
    spec = {k: v.shape for k, v in maps[0].items()}
    nc = build(cfg, spec, ALL_STAGES, {})
    res = run_bass_kernel_spmd(nc, maps, core_ids=list(range(NCORE)))
    out = np.zeros((2, cfg.seq, D), np.float32)
    for c in range(NCORE):
        b, q = c // 4, c % 4
        out[b, q * cfg.TL:(q + 1) * cfg.TL] = res.results[c]["out"]
    return out
```
